# Optimizing a Trainium2 kernel written in Bass

```python
import jax, jax.numpy as jnp
from jax import lax
import numpy as np

D_MODEL = 1024
BATCH = 8
SEQ = 8192
DEPTH = 2

MEM_LEN = 256
N_EVEN = (DEPTH + 1) // 2
N_ODD = DEPTH // 2
A_HEADS = 4
A_KDIM = 128
A_VDIM = 128
A_CHUNK = 64
A_FDIM = A_HEADS * A_KDIM
A_WIDTH = A_HEADS * A_VDIM
B_WIDTH = 512
B_GROUPS = 4
B_CONV = 3
C_HEADS = 8
C_HEAD_DIM = 64
C_Q_RANK = 256
C_KV_RANK = 128
C_IDX_HEADS = 8
C_IDX_DIM = 64
C_MAX_TOPK = 256
C_QBLOCK = 128
C_WIDTH = C_HEADS * C_HEAD_DIM
D_GROUPS = 4
D_CHUNK = 128
D_WIDTH = 512
D_GROUP_DIM = D_WIDTH // D_GROUPS
X_HEADS = 4
X_HEAD_DIM = D_MODEL // X_HEADS
FF_DIM = ((8 * D_MODEL // 3 + 255) // 256) * 256

EVEN_SPLITS = (A_FDIM, A_FDIM, A_WIDTH, A_WIDTH, B_WIDTH, B_WIDTH, B_WIDTH)
EVEN_IN = sum(EVEN_SPLITS)
ODD_SPLITS = (C_Q_RANK, C_KV_RANK, C_IDX_DIM, C_IDX_HEADS, D_WIDTH, D_WIDTH)
ODD_IN = sum(ODD_SPLITS)

kernel_name = 'hybrid_hgrn2_shortconv_dsa_gmlp_trunk'

F32 = jnp.float32


def rms_norm(x, g, eps=1e-6):
    xf = x.astype(F32)
    y = xf * lax.rsqrt(jnp.mean(xf * xf, axis=-1, keepdims=True) + eps)
    return (y * g.astype(F32)).astype(x.dtype)


def layer_norm(x, g, b, eps=1e-5):
    xf = x.astype(F32)
    mu = jnp.mean(xf, axis=-1, keepdims=True)
    var = jnp.mean(jnp.square(xf - mu), axis=-1, keepdims=True)
    y = (xf - mu) * lax.rsqrt(var + eps)
    return (y * g.astype(F32) + b.astype(F32)).astype(x.dtype)


def split_cols(t, sizes):
    out, start = [], 0
    for s in sizes:
        out.append(t[..., start:start + s])
        start += s
    return out


def hgrn2_mixer(q_raw, f_raw, i_raw, g_raw, lb, out_gain):
    bn, s, _ = q_raw.shape
    nc = s // A_CHUNK
    q = jax.nn.silu(q_raw.astype(F32))
    f = lb + (1.0 - lb) * jax.nn.sigmoid(f_raw.astype(F32))
    k = 1.0 - f
    logf = jnp.log(f)
    v = i_raw.astype(F32)

    def chunks(t, d):
        return t.reshape(bn, nc, A_CHUNK, A_HEADS, d).transpose(1, 0, 3, 2, 4)

    xs = (chunks(q, A_KDIM), chunks(k, A_KDIM), chunks(logf, A_KDIM), chunks(v, A_VDIM))
    causal = jnp.tril(jnp.ones((A_CHUNK, A_CHUNK), bool))

    def step(state, inp):
        qc, kc, lfc, vc = inp
        b = jnp.cumsum(lfc, axis=2)
        o_inter = jnp.einsum('bhck,bhkv->bhcv', qc * jnp.exp(b), state)
        diff = b[:, :, :, None, :] - b[:, :, None, :, :]
        decay = jnp.exp(jnp.where(causal[:, :, None], diff, -jnp.inf))
        attn = jnp.einsum('bhtk,bhsk,bhtsk->bhts', qc, kc, decay)
        o_intra = jnp.einsum('bhts,bhsv->bhtv', attn, vc)
        b_last = b[:, :, -1:, :]
        new_state = jnp.exp(b_last[:, :, 0, :])[..., None] * state + jnp.einsum(
            'bhsk,bhsv->bhkv', kc * jnp.exp(b_last - b), vc)
        return new_state, o_inter + o_intra

    state0 = jnp.zeros((bn, A_HEADS, A_KDIM, A_VDIM), F32)
    _, o = lax.scan(step, state0, xs)
    o = o.transpose(1, 0, 3, 2, 4).reshape(bn, s, A_HEADS, A_VDIM)
    gate = jax.nn.silu(g_raw.astype(F32)).reshape(bn, s, A_HEADS, A_VDIM)
    o = rms_norm(o, out_gain) * gate
    return o.reshape(bn, s, A_WIDTH)


def short_conv_mixer(b_gate, c_gate, h, w_conv):
    z = c_gate * h
    y = lax.conv_general_dilated(
        z, w_conv[:, None, :].astype(z.dtype), window_strides=(1,),
        padding=[(B_CONV - 1, 0)], dimension_numbers=('NWC', 'WIO', 'NWC'),
        feature_group_count=B_WIDTH)
    return b_gate * y


def dsa_mixer(c_q_raw, c_kv_raw, k_idx_raw, w_idx_raw, q_norm_g, kv_norm_g,
              w_uq, w_uk, w_uv, idx_wq, idx_k_g, idx_k_b):
    bn, s, _ = c_q_raw.shape
    topk = min(C_MAX_TOPK, s // 4)
    nb = s // C_QBLOCK
    cq = rms_norm(c_q_raw, q_norm_g)
    ckv = rms_norm(c_kv_raw, kv_norm_g).astype(F32)
    q = (cq @ w_uq).reshape(bn, s, C_HEADS, C_HEAD_DIM)
    q_lat = jnp.einsum('bshd,hdc->bshc', q, w_uk).astype(F32) * (C_HEAD_DIM ** -0.5)
    q_idx = (cq @ idx_wq).reshape(bn, s, C_IDX_HEADS, C_IDX_DIM).astype(F32) * (C_IDX_DIM ** -0.5)
    k_idx = layer_norm(k_idx_raw, idx_k_g, idx_k_b).astype(F32)
    w_idx = w_idx_raw.astype(F32) * (C_IDX_HEADS ** -0.5)

    def blocks(t):
        return jnp.moveaxis(t.reshape((bn, nb, C_QBLOCK) + t.shape[2:]), 1, 0)

    starts = jnp.arange(nb, dtype=jnp.int32) * C_QBLOCK
    key_pos = jnp.arange(s, dtype=jnp.int32)

    def one_block(args):
        qb, qib, wb, start = args
        t_pos = start + jnp.arange(C_QBLOCK, dtype=jnp.int32)
        logits = jnp.einsum('bthd,bsd->bths', qib, k_idx)
        score = jnp.einsum('bths,bth->bts', jax.nn.relu(logits), wb)
        causal = key_pos[None, :] <= t_pos[:, None]
        score = jnp.where(causal[None], score, -jnp.inf)
        _, idx = lax.top_k(score, topk)
        valid = idx <= t_pos[None, :, None]
        kv_sel = jax.vmap(lambda c, i: c[i])(ckv, idx)
        sc = jnp.einsum('bthc,btkc->bthk', qb, kv_sel)
        sc = jnp.where(valid[:, :, None, :], sc, -jnp.inf)
        p = jax.nn.softmax(sc, axis=-1)
        return jnp.einsum('bthk,btkc->bthc', p, kv_sel)

    o_lat = lax.map(one_block, (blocks(q_lat), blocks(q_idx), blocks(w_idx), starts))
    o_lat = jnp.moveaxis(o_lat, 0, 1).reshape(bn, s, C_HEADS, C_KV_RANK)
    o = jnp.einsum('bshc,hcd->bshd', o_lat, w_uv.astype(F32))
    return o.reshape(bn, s, C_WIDTH)


def spatial_gating_mixer(u_raw, v_raw, v_g, v_b, w_s, b_s):
    bn, s, _ = u_raw.shape
    nch = s // D_CHUNK
    u = jax.nn.gelu(u_raw, approximate=False)
    v = layer_norm(jax.nn.gelu(v_raw, approximate=False), v_g, v_b)
    v = v.reshape(bn, nch, D_CHUNK, D_GROUPS, D_GROUP_DIM)
    w_causal = w_s * jnp.tril(jnp.ones((D_CHUNK, D_CHUNK), w_s.dtype))
    mixed = jnp.einsum('gts,bnsgc->bntgc', w_causal, v) + b_s.T[:, :, None]
    return u * mixed.reshape(bn, s, D_WIDTH)


def memory_cross_attention(h, mem, mem_g, wq, wkv, wo):
    bn, s, _ = h.shape
    m_len = mem.shape[1]
    m = rms_norm(mem, mem_g)
    q = (h @ wq).reshape(bn, s, X_HEADS, X_HEAD_DIM)
    kv = (m @ wkv).reshape(bn, m_len, 2, X_HEADS, X_HEAD_DIM)
    k, v = kv[:, :, 0], kv[:, :, 1]
    sc = jnp.einsum('bshd,bmhd->bhsm', q, k).astype(F32) * (X_HEAD_DIM ** -0.5)
    p = jax.nn.softmax(sc, axis=-1).astype(v.dtype)
    o = jnp.einsum('bhsm,bmhd->bshd', p, v).reshape(bn, s, D_MODEL)
    return o @ wo


def swiglu_ffn(h, w13, w2):
    gu = h @ w13
    g, u = gu[..., :FF_DIM], gu[..., FF_DIM:]
    return (jax.nn.silu(g) * u) @ w2


def setup_inputs(seed: int = 0) -> dict:
    key = jax.random.key(seed)
    ks = iter(jax.random.split(key, 64))

    def nrm(shape, fan_in):
        return jax.random.normal(next(ks), shape, F32) * (fan_in ** -0.5)

    def gain(shape):
        return 1.0 + 0.02 * jax.random.normal(next(ks), shape, F32)

    def small(shape, scale=0.02):
        return scale * jax.random.normal(next(ks), shape, F32)

    return {
        'x': jax.random.normal(next(ks), (BATCH, SEQ, D_MODEL), F32),
        'mem': jax.random.normal(next(ks), (BATCH, MEM_LEN, D_MODEL), F32),
        'hgrn_lower_bounds': small((DEPTH + 1, A_FDIM), 0.1),
        'even_mix_norm': gain((N_EVEN, D_MODEL)),
        'even_w_in': nrm((N_EVEN, D_MODEL, EVEN_IN), D_MODEL),
        'even_a_out_norm': gain((N_EVEN, A_HEADS, A_VDIM)),
        'even_b_conv': nrm((N_EVEN, B_CONV, B_WIDTH), B_CONV),
        'even_w_out': nrm((N_EVEN, A_WIDTH + B_WIDTH, D_MODEL), A_WIDTH + B_WIDTH),
        'odd_mix_norm': gain((N_ODD, D_MODEL)),
        'odd_w_in': nrm((N_ODD, D_MODEL, ODD_IN), D_MODEL),
        'odd_c_q_norm': gain((N_ODD, C_Q_RANK)),
        'odd_c_kv_norm': gain((N_ODD, C_KV_RANK)),
        'odd_c_w_uq': nrm((N_ODD, C_Q_RANK, C_HEADS * C_HEAD_DIM), C_Q_RANK),
        'odd_c_w_uk': nrm((N_ODD, C_HEADS, C_HEAD_DIM, C_KV_RANK), C_KV_RANK),
        'odd_c_w_uv': nrm((N_ODD, C_HEADS, C_KV_RANK, C_HEAD_DIM), C_KV_RANK),
        'odd_c_idx_wq': nrm((N_ODD, C_Q_RANK, C_IDX_HEADS * C_IDX_DIM), C_Q_RANK),
        'odd_c_idx_k_g': gain((N_ODD, C_IDX_DIM)),
        'odd_c_idx_k_b': small((N_ODD, C_IDX_DIM)),
        'odd_d_v_g': gain((N_ODD, D_WIDTH)),
        'odd_d_v_b': small((N_ODD, D_WIDTH)),
        'odd_d_w_s': nrm((N_ODD, D_GROUPS, D_CHUNK, D_CHUNK), D_CHUNK),
        'odd_d_b_s': gain((N_ODD, D_GROUPS, D_CHUNK)),
        'odd_w_out': nrm((N_ODD, C_WIDTH + D_WIDTH, D_MODEL), C_WIDTH + D_WIDTH),
        'xa_norm': gain((DEPTH, D_MODEL)),
        'xa_mem_norm': gain((DEPTH, D_MODEL)),
        'xa_wq': nrm((DEPTH, D_MODEL, D_MODEL), D_MODEL),
        'xa_wkv': nrm((DEPTH, D_MODEL, 2 * D_MODEL), D_MODEL),
        'xa_wo': nrm((DEPTH, D_MODEL, D_MODEL), D_MODEL),
        'ffn_norm': gain((DEPTH, D_MODEL)),
        'ffn_w13': nrm((DEPTH, D_MODEL, 2 * FF_DIM), D_MODEL),
        'ffn_w2': nrm((DEPTH, FF_DIM, D_MODEL), FF_DIM),
        'final_norm': gain((D_MODEL,)),
    }


def reference(x, mem, hgrn_lower_bounds, even_mix_norm, even_w_in, even_a_out_norm,
              even_b_conv, even_w_out, odd_mix_norm, odd_w_in, odd_c_q_norm, odd_c_kv_norm,
              odd_c_w_uq, odd_c_w_uk, odd_c_w_uv, odd_c_idx_wq, odd_c_idx_k_g, odd_c_idx_k_b,
              odd_d_v_g, odd_d_v_b, odd_d_w_s, odd_d_b_s, odd_w_out, xa_norm, xa_mem_norm,
              xa_wq, xa_wkv, xa_wo, ffn_norm, ffn_w13, ffn_w2, final_norm):
    lower_bounds = jnp.cumsum(jax.nn.softmax(hgrn_lower_bounds.astype(F32), axis=0), axis=0)
    for layer in range(DEPTH):
        j = layer // 2
        if layer % 2 == 0:
            h = rms_norm(x, even_mix_norm[j])
            proj = h @ even_w_in[j]
            aq, af, ai, ag, bb, bc, bh = split_cols(proj, EVEN_SPLITS)
            o_a = hgrn2_mixer(aq, af, ai, ag, lower_bounds[layer], even_a_out_norm[j])
            o_b = short_conv_mixer(bb, bc, bh, even_b_conv[j])
            mixed = jnp.concatenate([o_a.astype(x.dtype), o_b.astype(x.dtype)], axis=-1) @ even_w_out[j]
        else:
            h = rms_norm(x, odd_mix_norm[j])
            proj = h @ odd_w_in[j]
            cq, ckv, kidx, widx, du, dv = split_cols(proj, ODD_SPLITS)
            o_c = dsa_mixer(cq, ckv, kidx, widx, odd_c_q_norm[j], odd_c_kv_norm[j],
                            odd_c_w_uq[j], odd_c_w_uk[j], odd_c_w_uv[j], odd_c_idx_wq[j],
                            odd_c_idx_k_g[j], odd_c_idx_k_b[j])
            o_d = spatial_gating_mixer(du, dv, odd_d_v_g[j], odd_d_v_b[j], odd_d_w_s[j], odd_d_b_s[j])
            mixed = jnp.concatenate([o_c.astype(x.dtype), o_d.astype(x.dtype)], axis=-1) @ odd_w_out[j]
        x = x + mixed.astype(x.dtype)
        x = x + memory_cross_attention(rms_norm(x, xa_norm[layer]), mem, xa_mem_norm[layer],
                                       xa_wq[layer], xa_wkv[layer], xa_wo[layer]).astype(x.dtype)
        x = x + swiglu_ffn(rms_norm(x, ffn_norm[layer]), ffn_w13[layer], ffn_w2[layer]).astype(x.dtype)
    return rms_norm(x, final_norm)
```

```python
import contextlib
import numpy as np
import concourse.bass as bass
import concourse.mybir as mybir
from concourse.bass_utils import run_bass_kernel_spmd

F32 = mybir.dt.float32
BF16 = mybir.dt.bfloat16
AF = mybir.ActivationFunctionType
ALU = mybir.AluOpType
AX = mybir.AxisListType

EPOCH = 30000
NSLOT = 16
D = 1024
T = 512
FF = 2816
NEG = -1.0e30
import os
STAGE = int(os.environ.get('KSTAGE', '9'))
SUB = int(os.environ.get('KSUB', '9'))
KL1 = int(os.environ.get('KL1', '9'))


class Prog:
    ENGS = ['tensor', 'vector', 'scalar', 'gpsimd', 'sync']

    def __init__(self, nc):
        self.nc = nc
        self.q = {e: [] for e in self.ENGS}
        self.cnt = {e: 0 for e in self.ENGS}
        self.dcnt = {e: 0 for e in self.ENGS}
        self.known = {e: {} for e in self.ENGS}
        self.last_w = {}
        self.readers = {}

    def _need(self, waits, ev, eng, raw):
        if ev is None:
            return
        kind, s, n = ev
        if kind == 'E' and s == eng:
            if eng == 'tensor':
                return
        k = (kind, s)
        if self.known[eng].get(k, 0) >= n:
            return
        if waits.get(k, 0) < n:
            waits[k] = n

    def emit(self, eng, fn, reads=(), writes=(), dma=False, extra=()):
        pr = [k for k in reads if isinstance(k, tuple) and k[0] == 'pb']
        if pr:
            reads = [k for k in reads if not (isinstance(k, tuple) and k[0] == 'pb')]
            writes = list(writes) + [k for k in pr if k not in writes]
        waits = {}
        for ev in extra:
            self._need(waits, ev, eng, True)
        for k in reads:
            self._need(waits, self.last_w.get(k), eng, True)
        for k in writes:
            self._need(waits, self.last_w.get(k), eng, False)
            for kk, n in self.readers.get(k, {}).items():
                self._need(waits, (kk[0], kk[1], n), eng, False)
        if dma:
            i = self.dcnt[eng]
            self.dcnt[eng] = i + 1
            slot = i % NSLOT
            n = i // NSLOT + 1
            if n > 1:
                self._need(waits, ('D', (eng, slot), n - 1), eng, False)
            ev = ('D', (eng, slot), n)
        else:
            n = self.cnt[eng] + 1
            self.cnt[eng] = n
            ev = ('E', eng, n)
        for k, n_ in waits.items():
            self.known[eng][k] = n_
        kk = (ev[0], ev[1])
        for k in reads:
            d = self.readers.setdefault(k, {})
            if d.get(kk, 0) < ev[2]:
                d[kk] = ev[2]
        for k in writes:
            self.last_w[k] = ev
            self.readers[k] = {}
        self.q[eng].append((list(waits.items()), fn, ev))
        return ev

    def all_events(self):
        evs = []
        for e in self.ENGS:
            if self.cnt[e] > 0:
                evs.append(('E', e, self.cnt[e]))
            for slot in range(min(NSLOT, self.dcnt[e])):
                evs.append(('D', (e, slot), (self.dcnt[e] - 1 - slot) // NSLOT + 1))
        return evs

    def barrier(self, dummies):
        evs = self.all_events()
        for eng, (fn, writes) in dummies.items():
            self.emit(eng, fn, (), writes, extra=evs)

    def finalize(self):
        nc = self.nc
        st = contextlib.ExitStack()
        esem = {}
        for e in self.ENGS:
            ne = self.cnt[e] // EPOCH + 1
            esem[e] = [st.enter_context(nc.semaphore(f"s_{e}_{j}")) for j in range(ne)]
        dsem = {}
        per = EPOCH // 16
        for e in self.ENGS:
            for slot in range(min(NSLOT, self.dcnt[e])):
                nd = (self.dcnt[e] + NSLOT - 1) // NSLOT
                ne = nd // per + 1
                dsem[(e, slot)] = [st.enter_context(nc.semaphore(f"d_{e}_{slot}_{j}")) for j in range(ne)]

        def hw(kind, s, n):
            if kind == 'E':
                return esem[s][(n - 1) // EPOCH], (n - 1) % EPOCH + 1
            return dsem[s][(n - 1) // per], ((n - 1) % per + 1) * 16

        with nc.Block() as block:
            def run(engname, engobj):
                for waits, fn, ev in self.q[engname]:
                    for (kind, s), n in waits:
                        sem, val = hw(kind, s, n)
                        engobj.wait_ge(sem, val)
                    inst = fn(engobj)
                    sem, val = hw(*ev)
                    inst.then_inc(sem, 16 if ev[0] == 'D' else 1)
                if engname == 'sync':
                    for e in self.ENGS:
                        if self.cnt[e] > 0:
                            sem, val = hw('E', e, self.cnt[e])
                            engobj.wait_ge(sem, val)
                        for slot in range(min(NSLOT, self.dcnt[e])):
                            nlast = (self.dcnt[e] - 1 - slot) // NSLOT + 1
                            sem, val = hw('D', (e, slot), nlast)
                            engobj.wait_ge(sem, val)

            @block.tensor
            def _(eng):
                run('tensor', eng)

            @block.vector
            def _(eng):
                run('vector', eng)

            @block.scalar
            def _(eng):
                run('scalar', eng)

            @block.gpsimd
            def _(eng):
                run('gpsimd', eng)

            @block.sync
            def _(eng):
                run('sync', eng)
        st.close()


WEIGHT_SPECS = [
    ('hgrn_lower_bounds', [3, 512]), ('even_mix_norm', [1, 1024]), ('even_w_in', [1, 1024, 3584]),
    ('even_a_out_norm', [1, 4, 128]), ('even_b_conv', [1, 3, 512]), ('even_w_out', [1, 1024, 1024]),
    ('odd_mix_norm', [1, 1024]), ('odd_w_in', [1, 1024, 1480]), ('odd_c_q_norm', [1, 256]),
    ('odd_c_kv_norm', [1, 128]), ('odd_c_w_uq', [1, 256, 512]), ('odd_c_w_uk', [1, 8, 64, 128]),
    ('odd_c_w_uv', [1, 8, 128, 64]), ('odd_c_idx_wq', [1, 256, 512]), ('odd_c_idx_k_g', [1, 64]),
    ('odd_c_idx_k_b', [1, 64]), ('odd_d_v_g', [1, 512]), ('odd_d_v_b', [1, 512]),
    ('odd_d_w_s', [1, 4, 128, 128]), ('odd_d_b_s', [1, 4, 128]), ('odd_w_out', [1, 1024, 1024]),
    ('xa_norm', [2, 1024]), ('xa_mem_norm', [2, 1024]), ('xa_wq', [2, 1024, 1024]),
    ('xa_wkv', [2, 1024, 2048]), ('xa_wo', [2, 1024, 1024]), ('ffn_norm', [2, 1024]),
    ('ffn_w13', [2, 1024, 5632]), ('ffn_w2', [2, 2816, 1024]), ('final_norm', [1024]),
]


def build(S, nlayers=2):
    NT = S // T
    nc = bass.Bass("TRN2", target_bir_lowering=False)
    P = Prog(nc)
    x_d = nc.dram_tensor("x", [S, D], F32, kind="ExternalInput").ap()
    mem_d = nc.dram_tensor("mem", [256, D], F32, kind="ExternalInput").ap()
    y_d = nc.dram_tensor("y", [S, D], F32, kind="ExternalOutput").ap()
    Wd = {}
    for name, shp in WEIGHT_SPECS:
        Wd[name] = nc.dram_tensor(name, shp, F32, kind="ExternalInput").ap()

    def sb(name, shape, dt):
        return nc.alloc_sbuf_tensor(name, shape, dt).ap()

    def mm(out, lhsT, rhs, start, stop, reads, writes, sgc=False):
        P.emit('tensor', lambda e: e.matmul(out, lhsT=lhsT, rhs=rhs, start=start, stop=stop, skip_group_check=sgc), reads, writes)

    def tr(out, in_, ident, reads, writes):
        P.emit('tensor', lambda e: e.transpose(out=out, in_=in_, identity=ident), reads, writes)

    def act(out, in_, func, reads, writes, scale=None, bias=None, accum=None):
        kw = {}
        if scale is not None:
            kw['scale'] = scale
        if bias is not None:
            kw['bias'] = bias
        if accum is not None:
            kw['accum_out'] = accum
        P.emit('scalar', lambda e: e.activation(out=out, in_=in_, func=func, **kw), reads, writes)

    def ts(eng, out, in0, s1, s2, op0, op1, reads, writes, accum=None):
        kw = {}
        if accum is not None:
            kw['accum_out'] = accum
        if out.dtype == mybir.dt.float8e4:
            kw['saturate'] = False
        if op1 is None:
            P.emit(eng, lambda e: e.tensor_scalar(out=out, in0=in0, scalar1=s1, scalar2=None, op0=op0, **kw), reads, writes)
        else:
            P.emit(eng, lambda e: e.tensor_scalar(out=out, in0=in0, scalar1=s1, scalar2=s2, op0=op0, op1=op1, **kw), reads, writes)

    def tt(eng, out, in0, in1, op, reads, writes):
        P.emit(eng, lambda e: e.tensor_tensor(out=out, in0=in0, in1=in1, op=op), reads, writes)

    def stt(out, in0, scalar, in1, op0, op1, reads, writes):
        P.emit('vector', lambda e: e.scalar_tensor_tensor(out=out, in0=in0, scalar=scalar, in1=in1, op0=op0, op1=op1), reads, writes)

    def cp(eng, out, in_, reads, writes):
        if eng == 'scalar':
            P.emit(eng, lambda e: e.copy(out=out, in_=in_), reads, writes)
        else:
            P.emit(eng, lambda e: e.tensor_copy(out=out, in_=in_), reads, writes)

    def memset(eng, ap, val, writes):
        P.emit(eng, lambda e: e.memset(ap, val), (), writes)

    def dma(eng, out, in_, reads, writes, slow=False):
        if slow:
            P.emit(eng, lambda e: e.dma_start(out=out, in_=in_, allow_slow_non_contiguous=True), reads, writes, dma=True)
        else:
            P.emit(eng, lambda e: e.dma_start(out=out, in_=in_), reads, writes, dma=True)

    wblocks = {}
    cast_i = [0]

    def make_block(key, pieces, kcn, ncols):
        wb = nc.dram_tensor("wb_" + key, [128, kcn, ncols], BF16).ap()
        pending[key] = (wb, pieces)
        wblocks[key] = (wb, kcn, ncols)

    pending = {}

    def emit_casts(order):
        for key in order:
            if key not in pending:
                continue
            wb, pieces = pending.pop(key)
            for src, c0 in pieces:
                n = src.shape[1]
                dma('gpsimd', wb[:, :, c0:c0 + n], src.rearrange("(kc p) n -> p kc n", p=128), (), [('wb', key)])

    for nb in range(7):
        make_block(f'ein{nb}', [(Wd['even_w_in'][0][:, nb * 512:(nb + 1) * 512], 0)], 8, 512)
    for nb in range(2):
        make_block(f'eout{nb}', [(Wd['even_w_out'][0][:, nb * 512:(nb + 1) * 512], 0)], 8, 512)
    for l in range(2):
        for nb in range(2):
            make_block(f'wq{l}_{nb}', [(Wd['xa_wq'][l][:, nb * 512:(nb + 1) * 512], 0)], 8, 512)
            make_block(f'wo{l}_{nb}', [(Wd['xa_wo'][l][:, nb * 512:(nb + 1) * 512], 0)], 8, 512)
        for nb in range(4):
            make_block(f'wkv{l}_{nb}', [(Wd['xa_wkv'][l][:, nb * 512:(nb + 1) * 512], 0)], 8, 512)
        for p_ in range(11):
            make_block(f'w13_{l}_{p_}', [(Wd['ffn_w13'][l][:, p_ * 256:(p_ + 1) * 256], 0),
                                         (Wd['ffn_w13'][l][:, FF + p_ * 256:FF + (p_ + 1) * 256], 256)], 8, 512)
        for half, (c0, groups) in enumerate([(0, [8, 2]), (10, [8, 4])]):
            for nb in range(2):
                k0 = c0
                for kg, kn in enumerate(groups):
                    make_block(f'w2_{l}_{half}_{nb}_{kg}', [(Wd['ffn_w2'][l][k0 * 128:(k0 + kn) * 128, nb * 512:(nb + 1) * 512], 0)], kn, 512)
                    k0 += kn
    if nlayers > 1:
        make_block('oin0', [(Wd['odd_w_in'][0][:, 0:448], 0), (Wd['odd_w_in'][0][:, 384:448], 448)], 8, 512)
        make_block('oin1', [(Wd['odd_w_in'][0][:, 456:968], 0)], 8, 512)
        make_block('oin2', [(Wd['odd_w_in'][0][:, 968:1480], 0)], 8, 512)
        for nb in range(2):
            make_block(f'oout{nb}', [(Wd['odd_w_out'][0][:, nb * 512:(nb + 1) * 512], 0)], 8, 512)

    ident = sb("ident", [128, 128], BF16)
    ones = sb("ones", [128, 128], BF16)
    xs = sb("xs", [128, 4, D], F32)
    xn = sb("xn", [128, 1, D], BF16)
    hT = sb("hT", [128, 8, T], BF16)
    A = sb("A", [128, 18, T], BF16)
    Fb = sb("F", [128, 8, T], F32)
    Vt = sb("Vt", [128, 4, T], BF16)
    KTt = sb("KTt", [128, 4, T], BF16)
    WR = sb("WR", [128, 4, 8, 512], BF16)
    gall = sb("gall", [128, 7, 8], F32)
    ss = sb("ss", [128, 4], F32)
    rs = sb("rs", [128, 4], F32)
    xaK = sb("xaK", [128, 2, 8, 256], BF16)
    xaV = sb("xaV", [128, 2, 2, D], BF16)
    Sst = sb("Sst", [128, 4, 128], F32)
    Sbf = sb("Sbf", [128, 4, 128], BF16)
    lbr = sb("lbr", [128, 4, 3], F32)
    lbs = sb("lbs", [128, 4], F32)
    lb = sb("lb", [128, 4], F32)
    oml = sb("oml", [128, 4], F32)
    again = sb("again", [128, 4], F32)
    convw = sb("convw", [128, 4, 3], F32)
    zc = sb("zc", [128, 4, T + 2], BF16)
    EBL = sb("EBL", [128, 4, 8], F32)
    mpair = sb("mpair", [128, 128], F32)
    rmask = sb("rmask", [128, T], BF16)
    attn_sb = sb("attn_sb", [128, 2, 128], BF16)
    osq = sb("osq", [128, T], BF16)
    PTx = sb("PTx", [128, 2, T], BF16)
    junk = PTx.rearrange("p a t -> p (a t)")
    JK = [('PTx', 0), ('PTx', 1)]

    pb = [nc.alloc_psum_tensor(f"pb{i}", [128, 512], F32).ap() for i in range(8)]
    pm_i = [0]

    PMB = (0, 1, 2)

    def pm_next():
        i = PMB[pm_i[0] % len(PMB)]
        pm_i[0] += 1
        return pb[i], ('pb', i)

    def pm4():
        return [(pb[i], ('pb', i)) for i in range(4)]

    pt_i = [0]

    def pt_next():
        i = 4 + pt_i[0] % 2
        pt_i[0] += 1
        return pb[i].bitcast(BF16), ('pb', i)

    memset('gpsimd', ident, 0.0, ['ident'])
    P.emit('gpsimd', lambda e: e.affine_select(out=ident, in_=ident, pattern=[[-1, 128]], compare_op=ALU.not_equal,
                                               fill=1.0, base=0, channel_multiplier=1), ['ident'], ['ident'])
    memset('vector', ones, 1.0, ['ones'])
    memset('vector', mpair, 1.0, ['mpair'])
    P.emit('gpsimd', lambda e: e.affine_select(out=mpair, in_=mpair, pattern=[[1, 128]], compare_op=ALU.is_ge,
                                               fill=0.0, base=0, channel_multiplier=-1), ['mpair'], ['mpair'])
    memset('gpsimd', mpair[0:64, 64:128], 0.0, ['mpair'])
    memset('vector', rmask, 1.0, ['rmask'])
    memset('vector', rmask.rearrange("p (c s) -> p c s", s=64)[:, :, 0:1], 0.0, ['rmask'])
    memset('vector', zc, 0.0, [('zc', c) for c in range(4)])
    memset('vector', Sst, 0.0, [('S', h) for h in range(4)])
    memset('vector', Sbf, 0.0, [('Sbf', h) for h in range(4)])

    gsrc = [Wd['even_mix_norm'][0], Wd['xa_norm'][0], Wd['ffn_norm'][0], Wd['odd_mix_norm'][0],
            Wd['xa_norm'][1], Wd['ffn_norm'][1], Wd['final_norm']]
    for i, g in enumerate(gsrc):
        dma('sync', gall[:, i, :], g.rearrange("(kc p) -> p kc", p=128), (), ['gall'], slow=True)
    G_EMIX, G_XA0, G_FFN0, G_OMIX, G_XA1, G_FFN1, G_FIN = range(7)
    for h in range(4):
        dma('sync', lbr[:, h, :], Wd['hgrn_lower_bounds'][:, h * 128:(h + 1) * 128].rearrange("s k -> k s"), (), ['lbr'], slow=True)
    dma('sync', again, Wd['even_a_out_norm'][0].rearrange("h v -> v h"), (), ['again'], slow=True)
    for c in range(4):
        dma('sync', convw[:, c, :], Wd['even_b_conv'][0][:, c * 128:(c + 1) * 128].rearrange("k p -> p k"), (), ['convw'], slow=True)
    act(lbr, lbr, AF.Exp, ['lbr'], ['lbr'])
    P.emit('vector', lambda e: e.reduce_sum(out=lbs, in_=lbr, axis=AX.X), ['lbr'], ['lbs'])
    P.emit('vector', lambda e: e.reciprocal(out=lbs, in_=lbs), ['lbs'], ['lbs'])
    tt('vector', lb, lbr[:, :, 0], lbs, ALU.mult, ['lbr', 'lbs'], ['lb'])
    ts('vector', oml, lb, -1.0, 1.0, ALU.mult, ALU.add, ['lb'], ['oml'])

    wseq = []
    wstate = {'pos': 0, 'loaded': 0, 'dry': True}

    def wload(m):
        key = wseq[m]
        wb, kcn, ncols = wblocks[key]
        slot = m % 4
        dma('sync', WR[:, slot, 0:kcn, 0:ncols], wb, [('wb', key)], [('W', slot)])

    def wnext(key, keep_prev=False):
        i = wstate['pos']
        wstate['pos'] = i + 1
        if wstate['dry']:
            wseq.append(key)
            return None, None
        assert wseq[i] == key, (i, wseq[i], key)
        lim = min(len(wseq) - 1, i + (1 if keep_prev else 2))
        while wstate['loaded'] <= lim:
            wload(wstate['loaded'])
            wstate['loaded'] += 1
        slot = i % 4
        return WR[:, slot], ('W', slot)

    def norm_T(src, nsub, gidx, dst, dkeys, skeys):
        for j in range(nsub):
            act(junk, src[:, j, :], AF.Square, [skeys[j]], JK + [('ss', j)], accum=ss[:, j:j + 1])
        act(rs[:, 0:nsub], ss[:, 0:nsub], AF.Sqrt, [('ss', j) for j in range(nsub)], ['rs'], scale=1.0 / D, bias=epsD)
        P.emit('vector', lambda e: e.reciprocal(out=rs[:, 0:nsub], in_=rs[:, 0:nsub]), ['rs'], ['rs'])
        for j in range(nsub):
            act(xn[:, 0, :], src[:, j, :], AF.Copy, [skeys[j], 'rs'], [('xn', 0)], scale=rs[:, j:j + 1])
            p, pk = pt_next()
            for kc in range(8):
                tr(p[:, kc * 128:(kc + 1) * 128], xn[:, 0, kc * 128:(kc + 1) * 128], ident, [('xn', 0), 'ident'], [pk])
            tt('vector', dst[:, :, j * 128:(j + 1) * 128], p.rearrange("p (k t) -> p k t", t=128),
               gall[:, gidx, :].unsqueeze(2).to_broadcast([128, 8, 128]), ALU.mult, [pk, 'gall'], dkeys)

    HTK = [('hT', kc) for kc in range(8)]
    XK = [('x', j) for j in range(4)]

    def proj_fm(wv, wk, col0, handler_bank):
        bank, bk = pm_next()
        for kc in range(8):
            mm(bank, wv[:, kc, col0:col0 + 128], hT[:, kc, :], kc == 0, kc == 7, [wk] + HTK, [bk])
        return bank, bk

    def resid_add(wkeys_fn, inT, inkeys, nkc_groups):
        for nb in range(2):
            banks = pm4()
            kc0 = 0
            ng = len(nkc_groups)
            for kg in range(ng):
                wv, wk = wnext(wkeys_fn(nb, kg))
                if wstate['dry']:
                    continue
                kn = nkc_groups[kg]
                for j in range(4):
                    bank, bk = banks[j]
                    for kc in range(kn):
                        mm(bank, inT[:, kc0 + kc, j * 128:(j + 1) * 128], wv[:, kc, :], kc0 + kc == 0,
                           (kg == ng - 1 and kc == kn - 1), [wk] + inkeys, [bk])
                kc0 += kn
            if wstate['dry']:
                continue
            for j in range(4):
                bank, bk = banks[j]
                tt('vector', xs[:, j, nb * 512:(nb + 1) * 512], xs[:, j, nb * 512:(nb + 1) * 512], bank, ALU.add,
                   [bk, ('x', j)], [('x', j)])

    def F(i):
        return Fb[:, i, :], ('F', i)

    epsD = sb("epsD", [128, 1], F32)
    memset('vector', epsD, 1e-6, ['eps'])

    def conv_branch(mix):
        dry = wstate['dry']
        zh = lambda c: (A[:, 8 + c, :], ('A', 8 + c))
        w_bh, k_bh = wnext('ein6')
        if not dry:
            for c in range(4):
                bank, bk = proj_fm(w_bh, k_bh, c * 128, None)
                zv, zk = zh(c)
                cp('scalar', zv, bank, [bk], [zk])
        w_bc, k_bc = wnext('ein5')
        if not dry:
            for c in range(4):
                bank, bk = proj_fm(w_bc, k_bc, c * 128, None)
                zv, zk = zh(c)
                tt('vector', zc[:, c, 2:T + 2], bank, zv, ALU.mult, [bk, zk], [('zc', c)])
        w_bb, k_bb = wnext('ein4')
        if not dry:
            for c in range(4):
                bank, bk = proj_fm(w_bb, k_bb, c * 128, None)
                f3, f3k = F(3); f4, f4k = F(4)
                ts('vector', f3, zc[:, c, 0:T], convw[:, c, 0:1], None, ALU.mult, None, [('zc', c), 'convw'], [f3k])
                stt(f4, zc[:, c, 1:T + 1], convw[:, c, 1:2], f3, ALU.mult, ALU.add, [('zc', c), 'convw', f3k], [f4k])
                stt(f3, zc[:, c, 2:T + 2], convw[:, c, 2:3], f4, ALU.mult, ALU.add, [('zc', c), 'convw', f4k], [f3k])
                mv, mk = mix(4 + c)
                tt('vector', mv, bank, f3, ALU.mult, [bk, f3k], [mk])
                cp('gpsimd', zc[:, c, 0:2], zc[:, c, T:T + 2], [('zc', c)], [('zc', c)])

    def xattn(l, gidx):
        dry = wstate['dry']
        if not dry:
            norm_T(xs, 4, gidx, hT, HTK, XK)
        qT = lambda c: (A[:, c, :], ('A', c))
        oT = lambda c: (A[:, 8 + c, :], ('A', 8 + c))
        for nb in range(2):
            wv, wk = wnext(f'wq{l}_{nb}')
            if dry:
                continue
            for c in range(4):
                bank, bk = proj_fm(wv, wk, c * 128, None)
                qv, qk = qT(nb * 4 + c)
                cp('scalar', qv, bank, [bk], [qk])
        if not dry:
            for hd in range(4):
                for mj in range(2):
                    bank, bk = pm_next()
                    for dc in range(2):
                        qv, qk = qT(hd * 2 + dc)
                        mm(bank, xaK[:, l, hd * 2 + dc, mj * 128:(mj + 1) * 128], qv, dc == 0, dc == 1, [('xaK', l), qk], [bk])
                    act(PTx[:, mj, :], bank, AF.Exp, [bk], [('PTx', mj)], scale=1.0 / 16.0)
                pa, pak = pb[6 + hd % 2], ('pb', 6 + hd % 2)
                for mj in range(2):
                    mm(pa, ones, PTx[:, mj, :], mj == 0, mj == 1, ['ones', ('PTx', mj)], [pak])
                f2, f2k = F(2 + hd % 2)
                P.emit('vector', lambda e, f2=f2, pa=pa: e.reciprocal(out=f2, in_=pa), [pak], [f2k])
                for dc in range(2):
                    bank, bk = pm_next()
                    for mj in range(2):
                        mm(bank, xaV[:, l, mj, hd * 256 + dc * 128: hd * 256 + (dc + 1) * 128], PTx[:, mj, :], mj == 0, mj == 1,
                           [('xaV', l), ('PTx', mj)], [bk])
                    ov, ok = oT(hd * 2 + dc)
                    tt('vector', ov, bank, f2, ALU.mult, [bk, f2k], [ok])
        resid_add(lambda nb, kg: f'wo{l}_{nb}', A[:, 8:16, :], [('A', 8 + c) for c in range(8)], [8])

    def ffn(l, gidx):
        dry = wstate['dry']
        if not dry:
            norm_T(xs, 4, gidx, hT, HTK, XK)
        for half, (p0, p1, groups) in enumerate([(0, 5, [8, 2]), (5, 11, [8, 4])]):
            for p_ in range(p0, p1):
                wv, wk = wnext(f'w13_{l}_{p_}')
                if dry:
                    continue
                for cc in range(2):
                    bg, bgk = proj_fm(wv, wk, cc * 128, None)
                    bu, buk = proj_fm(wv, wk, 256 + cc * 128, None)
                    f, fk = F(4 + (p_ * 2 + cc) % 4)
                    act(f, bg, AF.Silu, [bgk], [fk])
                    c = (p_ - p0) * 2 + cc
                    tt('vector', A[:, c, :], bu, f, ALU.mult, [buk, fk], [('A', c)])
            nch = sum(groups)
            resid_add(lambda nb, kg, half=half: f'w2_{l}_{half}_{nb}_{kg}', A[:, 0:nch, :], [('A', c) for c in range(nch)], groups)

    def mem_kv(l):
        ms = xs
        dma('sync', ms[:, 0:2, :], mem_d.rearrange("(j p) d -> p j d", p=128), (), [('x', 0), ('x', 1)])
        dma('sync', gall[:, 6, :], Wd['xa_mem_norm'][l].rearrange("(kc p) -> p kc", p=128), (), ['gall'], slow=True)
        norm_T(ms, 2, 6, hT, HTK, XK)
        for nb in range(4):
            wb, kcn, ncols = wblocks[f'wkv{l}_{nb}']
            dma('sync', WR[:, nb % 4, :, :], wb, [('wb', f'wkv{l}_{nb}')], [('W', nb % 4)])
        for oc in range(8):
            bank, bk = pm_next()
            for kc in range(8):
                mm(bank[:, 0:256], WR[:, oc // 4, kc, (oc % 4) * 128:(oc % 4 + 1) * 128], hT[:, kc, 0:256], kc == 0, kc == 7,
                   [('W', oc // 4)] + HTK, [bk])
            cp('scalar', xaK[:, l, oc, :], bank[:, 0:256], [bk], [('xaK', l)])
        for mj in range(2):
            for nb in range(2):
                bank, bk = pm_next()
                for kc in range(8):
                    mm(bank, hT[:, kc, mj * 128:(mj + 1) * 128], WR[:, 2 + nb, kc, :], kc == 0, kc == 7, [('W', 2 + nb)] + HTK, [bk])
                cp('scalar', xaV[:, l, mj, nb * 512:(nb + 1) * 512], bank, [bk], [('xaV', l)])

    def mark(label):
        if not wstate['dry']:
            MARKS.append((label, P.cnt['tensor'], P.cnt['vector'], P.cnt['scalar']))

    def tile_body(ti):
        dry = wstate['dry']
        mark('tile_start')
        mixf = lambda c: (A[:, c, :], ('A', c))
        if not dry:
            dma('sync', xs, x_d[ti * T:(ti + 1) * T, :].rearrange("(j p) d -> p j d", p=128), (), XK)
            norm_T(xs, 4, G_EMIX, hT, HTK, XK)
        mark('l0_norm_done')
        if STAGE >= 1:
            layer0_mixer_main()
        mark('l0_hgrn_done')
        if STAGE >= 2:
            conv_branch(mixf)
            resid_add(lambda nb, kg: f'eout{nb}', A[:, 0:8, :], [('A', c) for c in range(8)], [8])
        mark('l0_conv_eout_done')
        if STAGE >= 3:
            xattn(0, G_XA0)
        mark('l0_xa_done')
        if STAGE >= 4:
            ffn(0, G_FFN0)
        mark('l0_ffn_done')
        if nlayers > 1:
            layer1(ti)
            mark('l1_mixer_done')
            if os.environ.get('KSTOP', '0') == '1':
                return
            xattn(1, G_XA1)
            mark('l1_xa_done')
            ffn(1, G_FFN1)
            mark('l1_ffn_done')
        if not dry:
            for j in range(4):
                act(junk, xs[:, j, :], AF.Square, [('x', j)], JK + [('ss', j)], accum=ss[:, j:j + 1])
            act(rs, ss, AF.Sqrt, [('ss', j) for j in range(4)], ['rs'], scale=1.0 / D, bias=epsD)
            P.emit('vector', lambda e: e.reciprocal(out=rs, in_=rs), ['rs'], ['rs'])
            dma('sync', gfin, Wd['final_norm'].partition_broadcast(128), (), [('F', 0), ('F', 1)])
            for j in range(4):
                stt(xs[:, j, :], xs[:, j, :], rs[:, j:j + 1], gfin, ALU.mult, ALU.mult, [('x', j), 'rs', ('F', 0), ('F', 1)], [('x', j)])
            dma('sync', y_d[ti * T:(ti + 1) * T, :].rearrange("(j p) d -> p j d", p=128), xs, XK, [('y', ti)])

    def layer0_mixer_main():
        dry = wstate['dry']
        _l0()

    def _l0():
        dry = wstate['dry']
        qd = lambda h: (A[:, 0 + h, :], ('A', 0 + h))
        kd = lambda h: (A[:, 4 + h, :], ('A', 4 + h))
        kdl = lambda h: (A[:, 8 + h, :], ('A', 8 + h))
        og = lambda h: (A[:, 12 + h, :], ('A', 12 + h))
        mix = lambda c: (A[:, c, :], ('A', c))
        w_aq, k_aq = wnext('ein0')
        w_af, k_af = wnext('ein1', keep_prev=True)
        if not dry:
            for h in range(4):
                bank, bk = proj_fm(w_af, k_af, h * 128, None)
                f0, f0k = F(0); f1, f1k = F(1); f2, f2k = F(2); f3, f3k = F(3)
                f4, f4k = F(4); f5, f5k = F(5); f6, f6k = F(6)
                act(f0, bank, AF.Sigmoid, [bk], [f0k])
                ts('vector', f1, f0, oml[:, h:h + 1], lb[:, h:h + 1], ALU.mult, ALU.add, [f0k, 'oml', 'lb'], [f1k])
                act(f2, f1, AF.Ln, [f1k], [f2k])
                ts('vector', f3, f1, -1.0, 1.0, ALU.mult, ALU.add, [f1k], [f3k])
                P.emit('vector', lambda e, f4=f4, f2=f2: e.tensor_tensor_scan(out=f4, data0=rmask, data1=f2, initial=0.0,
                                                                             op0=ALU.mult, op1=ALU.add), [f2k, 'rmask'], [f4k])
                act(f5, f4, AF.Exp, [f4k], [f5k])
                act(f6, f4, AF.Exp, [f4k], [f6k], scale=-1.0)
                kdv, kdk = kd(h)
                tt('vector', kdv, f3, f6, ALU.mult, [f3k, f6k], [kdk])
                kdlv, kdlk = kdl(h)
                ebl_b = f5.rearrange("p (c s) -> p c s", s=64)[:, :, 63:64].to_broadcast([128, 8, 64])
                tt('vector', kdlv.rearrange("p (c s) -> p c s", s=64), kdv.rearrange("p (c s) -> p c s", s=64), ebl_b,
                   ALU.mult, [kdk, f5k], [kdlk])
                cp('gpsimd', EBL[:, h, :], f5.rearrange("p (c s) -> p c s", s=64)[:, :, 63], [f5k], [('EBL', h)])
                bank, bk = proj_fm(w_aq, k_aq, h * 128, None)
                act(f0, bank, AF.Silu, [bk], [f0k])
                qdv, qdk = qd(h)
                tt('vector', qdv, f0, f5, ALU.mult, [f0k, f5k], [qdk])
        w_ai, k_ai = wnext('ein2')
        if not dry:
            for j in range(4):
                bank, bk = pm_next()
                for kc in range(8):
                    mm(bank, hT[:, kc, j * 128:(j + 1) * 128], w_ai[:, kc, :], kc == 0, kc == 7, [k_ai] + HTK, [bk])
                cp('scalar', Vt[:, j, :], bank, [bk], [('Vt', j)])
        w_ag, k_ag = wnext('ein3')
        if not dry and SUB >= 2:
            for h in range(4):
                bank, bk = proj_fm(w_ag, k_ag, h * 128, None)
                ogv, ogk = og(h)
                act(ogv, bank, AF.Silu, [bk], [ogk])
            for j in range(4):
                p, pk = pt_next()
                for h in range(4):
                    kdlv, kdlk = kdl(h)
                    tr(p[:, h * 128:(h + 1) * 128], kdlv[:, j * 128:(j + 1) * 128], ident, [kdlk, 'ident'], [pk])
                cp('scalar', KTt[:, j, :], p[:, 0:512], [pk], [('KTt', j)])
            obanks = pm4()
            for j in range(4 if SUB >= 3 else 0):
                for h in range(4):
                    qdv, qdk = qd(h)
                    kdv, kdk = kd(h)
                    ob, obk = obanks[h]
                    par = (j * 4 + h) % 2
                    pa, pak = pb[6 + par], ('pb', 6 + par)
                    sl = slice(j * 128, (j + 1) * 128)
                    mm(pa[:, 0:128], kdv[:, sl], qdv[:, sl], True, True, [kdk, qdk], [pak])
                    asb = attn_sb[:, par, :]
                    ask = ('attn', par)
                    tt('vector', asb, pa[:, 0:128], mpair, ALU.mult, [pak, 'mpair'], [ask])
                    vsl = slice(h * 128, (h + 1) * 128)
                    mm(ob[:, sl], Vt[:, j, vsl], asb, True, False, [('Vt', j), ask], [obk])
                    for cc in range(2):
                        ps = slice(cc * 64, (cc + 1) * 64)
                        tsl = slice(j * 128 + cc * 64, j * 128 + (cc + 1) * 64)
                        mm(ob[:, tsl], Sbf[:, h, :], qdv[:, tsl], False, cc == 1, [('Sbf', h), qdk], [obk])
                        mm(pa[:, 128 + cc * 128:256 + cc * 128], KTt[ps, j, vsl], Vt[ps, j, vsl], True, True,
                           [('KTt', j), ('Vt', j)], [pak])
                        stt(Sst[:, h, :], Sst[:, h, :], EBL[:, h, j * 2 + cc:j * 2 + cc + 1], pa[:, 128 + cc * 128:256 + cc * 128],
                            ALU.mult, ALU.add, [('S', h), ('EBL', h), pak], [('S', h)])
                        cp('scalar', Sbf[:, h, :], Sst[:, h, :], [('S', h)], [('Sbf', h)])
            for h in range(4 if SUB >= 4 else 0):
                ob, obk = obanks[h]
                act(osq, ob, AF.Square, [obk], ['osq'])
                f1, f1k = F(1); f2, f2k = F(2)
                ogv, ogk = og(h)
                tt('vector', f1, ob, ogv, ALU.mult, [obk, ogk], [f1k])
                pa, pak = pb[6 + h % 2], ('pb', 6 + h % 2)
                mm(pa, ones, osq, True, True, ['ones', 'osq'], [pak])
                act(f2, pa, AF.Sqrt, [pak], [f2k], scale=1.0 / 128, bias=epsD)
                P.emit('vector', lambda e, f2=f2: e.reciprocal(out=f2, in_=f2), [f2k], [f2k])
                mv, mk = mix(h)
                stt(mv, f1, again[:, h:h + 1], f2, ALU.mult, ALU.mult, [f1k, 'again', f2k], [mk])


    if nlayers > 1:
        kidxT2 = sb("kidxT2", [128, S], BF16)
        ckvT = sb("ckvT", [128, S], BF16)
        ckvtok = sb("ckvtok", [128, S // 128, 128], BF16)
        score = sb("score", [128, S], BF16)
        mk = Fb.rearrange("p a t -> p (a t)").bitcast(BF16)
        FALL = [('F', i) for i in range(8)]
        wuq_sb = sb("wuq_sb", [128, 2, 512], BF16)
        iwq_sb = sb("iwq_sb", [128, 2, 512], BF16)
        wuk_sb = sb("wuk_sb", [128, 4, 128], BF16)
        wuv_sb = sb("wuv_sb", [128, 8, 64], BF16)
        widx_sb = sb("widx_sb", [128, 8, 8], BF16)
        wT_sb = sb("wT_sb", [128, 4, 128], BF16)
        wsl = sb("wsl", [128, 4, 128], BF16)
        bs_bc = sb("bs_bc", [128, 4, 128], BF16)
        vg_bc = sb("vg_bc", [128, 512], BF16)
        vb_bc = sb("vb_bc", [128, 512], BF16)
        cb = sb("cb", [128, 128], F32)
        cqg = sb("cqg", [128, 2], F32)
        ckvg = sb("ckvg", [128, 1], F32)
        kig = sb("kig", [128, 1], F32)
        kib = sb("kib", [128, 1], F32)
        eps5 = sb("eps5", [128, 1], F32)
        wraw = sb("wraw", [128, 4, 8], F32)
        wabs = sb("wabs", [128, 4, 8], F32)
        wsgn = sb("wsgn", [128, 4, 8], F32)
        sm = sb("sm", [128, 16], F32)
        lnst = sb("lnst", [128, 4, 4], F32)
        rden = sb("rden", [128, 8], F32)
        memset('vector', eps5, 1e-5, ['eps5'])
        dma('gpsimd', wuq_sb, Wd['odd_c_w_uq'][0].rearrange("(c p) n -> p c n", p=128), (), ['wuq'])
        dma('gpsimd', iwq_sb, Wd['odd_c_idx_wq'][0].rearrange("(c p) n -> p c n", p=128), (), ['iwq'])
        for hp in range(4):
            dma('gpsimd', wuk_sb[:, hp, :], Wd['odd_c_w_uk'][0][2 * hp:2 * hp + 2].rearrange("h d c -> (h d) c"), (), ['wuk'])
        dma('gpsimd', wuv_sb, Wd['odd_c_w_uv'][0].rearrange("h c d -> c h d"), (), ['wuv'])
        dma('gpsimd', widx_sb, Wd['odd_w_in'][0][:, 448:456].rearrange("(kc p) n -> p kc n", p=128), (), ['widx'], slow=True)
        dma('gpsimd', wsl, Wd['odd_d_w_s'][0].rearrange("g t s -> t g s"), (), ['wsl'])
        for g_ in range(4):
            P.emit('gpsimd', lambda e, g_=g_: e.affine_select(out=wsl[:, g_, :], in_=wsl[:, g_, :], pattern=[[-1, 128]], compare_op=ALU.is_ge,
                                                            fill=0.0, base=0, channel_multiplier=1), ['wsl'], ['wsl'])
        p_, pk_ = pt_next()
        for g_ in range(4):
            tr(p_[:, g_ * 128:(g_ + 1) * 128], wsl[:, g_, :], ident, ['wsl', 'ident'], [pk_])
        cp('vector', wT_sb, p_[:, 0:512].rearrange("p (g t) -> p g t", t=128), [pk_], ['wT'])
        dma('gpsimd', bs_bc.rearrange("p g t -> p (g t)"), Wd['odd_d_b_s'][0].rearrange("g t -> (g t)").partition_broadcast(128), (), ['bsbc'])
        dma('gpsimd', vg_bc, Wd['odd_d_v_g'][0].partition_broadcast(128), (), ['vgbc'])
        dma('gpsimd', vb_bc, Wd['odd_d_v_b'][0].partition_broadcast(128), (), ['vbbc'])
        memset('vector', cb, 0.0, ['cb'])
        P.emit('gpsimd', lambda e: e.affine_select(out=cb, in_=cb, pattern=[[-1, 128]], compare_op=ALU.is_ge,
                                                   fill=NEG, base=0, channel_multiplier=1), ['cb'], ['cb'])
        dma('sync', cqg, Wd['odd_c_q_norm'][0].rearrange("(c p) -> p c", p=128), (), ['cqg'], slow=True)
        dma('sync', ckvg, Wd['odd_c_kv_norm'][0].rearrange("(c p) -> p c", p=128), (), ['ckvg'], slow=True)
        for hh in range(2):
            dma('sync', kig[hh * 64:(hh + 1) * 64, :], Wd['odd_c_idx_k_g'][0].rearrange("(c p) -> p c", p=64), (), ['kig'], slow=True)
            dma('sync', kib[hh * 64:(hh + 1) * 64, :], Wd['odd_c_idx_k_b'][0].rearrange("(c p) -> p c", p=64), (), ['kib'], slow=True)

    NR = 16

    MBDT = mybir.dt.float8e4 if os.environ.get('KMB', 'fp8') == 'fp8' else BF16
    MBV = -224.0
    if nlayers > 1:
        if MBDT == BF16:
            assert S <= 4096
            mbv_ = Fb.rearrange("p a t -> p (a t)").bitcast(BF16)
            mbuf = [mbv_[:, 0:4096], mbv_[:, 4096:8192]]
        else:
            mbv_ = Fb.rearrange("p a t -> p (a t)").bitcast(MBDT)
            mbuf = [mbv_[:, 0:8192], mbv_[:, 8192:16384]]
        MBK = [[('F', i) for i in range(4)], [('F', 4 + i) for i in range(4)]]
        PTb = [hT[:, 0:2, :].rearrange("p a t -> p (a t)"), KTt[:, 0:2, :].rearrange("p a t -> p (a t)")]
        PTKb = [[('hT', 0), ('hT', 1)], [('KTt', 0), ('KTt', 1)]]
        acc_ = hT[:, 2:4, :].rearrange("p a t -> p (a t)").bitcast(F32)
        ACK = [('hT', 2), ('hT', 3)]
        Rh = [hT[:, 4, :], hT[:, 5, :]]
        RHK = [('hT', 4), ('hT', 5)]
        olat = hT[:, 6:8, :].rearrange("p a t -> p (a t)")
        OLK = [('hT', 6), ('hT', 7)]
        qlb = Vt[:, 0:2, :].rearrange("p a t -> p (a t)")
        QLK = [('Vt', 0), ('Vt', 1)]
        otok = Vt[:, 2, :]
        OTK = ('Vt', 2)
        ib_i = [0]
        qk_i = [0]

    def ib_next():
        i = ib_i[0] % 2
        ib_i[0] += 1
        return pb[i], ('pb', i)

    def qk_next():
        i = (2, 4, 5)[qk_i[0] % 3]
        qk_i[0] += 1
        return pb[i], ('pb', i)

    def dsa_indexer(ti, qbl):
        Q = ti * 4 + qbl
        L = (Q + 1) * 128
        qsl = slice(qbl * 128, (qbl + 1) * 128)
        nsb = (L + 511) // 512
        for sbi in range(nsb):
            n = min(512, L - sbi * 512)
            ksl = slice(sbi * 512, sbi * 512 + n)
            kk = [('kidx', sbi)]
            for h in range(8):
                bank, bk = ib_next()
                hp = slice((h % 2) * 64, (h % 2 + 1) * 64)
                mm(bank[:, 0:n], A[hp, 6 + h // 2, qsl], kidxT2[hp, ksl], True, True, [('A', 6 + h // 2)] + kk, [bk])
                r = Rh[h % 2]
                rk = RHK[h % 2]
                act(r[:, 0:n], bank[:, 0:n], AF.Relu, [bk, 'wabs'], [rk], scale=wabs[:, qbl, h:h + 1])
                if h == 0:
                    ts('vector', acc_[:, 0:n], r[:, 0:n], wsgn[:, qbl, 0:1], None, ALU.mult, None, [rk, 'wsgn'], ACK)
                elif h < 7:
                    stt(acc_[:, 0:n], r[:, 0:n], wsgn[:, qbl, h:h + 1], acc_[:, 0:n], ALU.mult, ALU.add, [rk, 'wsgn'] + ACK, ACK)
                else:
                    stt(score[:, ksl], r[:, 0:n], wsgn[:, qbl, h:h + 1], acc_[:, 0:n], ALU.mult, ALU.add, [rk, 'wsgn'] + ACK, ['score'])
        dsl = slice(Q * 128, (Q + 1) * 128)
        tt('vector', score[:, dsl], score[:, dsl], cb, ALU.add, ['score', 'cb'], ['score'])

    def dsa_threshold(ti, qbl):
        Q = ti * 4 + qbl
        L = (Q + 1) * 128
        mb = mbuf[Q % 2]
        mbk = MBK[Q % 2]
        lo = sm[:, 0:1]; hi = sm[:, 1:2]; w0_ = sm[:, 2:3]; mid = sm[:, 3:4]; cnt = sm[:, 4:5]; selw = sm[:, 5:6]
        if Q >= 2:
            P.emit('vector', lambda e: e.tensor_reduce(out=hi, in_=score[:, 0:L], axis=AX.X, op=ALU.max), ['score'], ['hi'])
            P.emit('vector', lambda e: e.tensor_reduce(out=lo, in_=score[:, 0:L - 128], axis=AX.X, op=ALU.min), ['score'], ['lo'])
            tt('vector', w0_, hi, lo, ALU.subtract, ['hi', 'lo'], ['w0'])
            for r_ in range(NR):
                sc_ = 2.0 ** -(r_ + 1)
                stt(mid, w0_, sc_, lo, ALU.mult, ALU.add, ['w0', 'lo'], ['mid'])
                ts('vector', mb[:, 0:L], score[:, 0:L], mid, 0.0, ALU.is_ge, ALU.add, ['score', 'mid'], mbk + ['cnt'], accum=cnt)
                ts('vector', selw, cnt, 256.0, sc_, ALU.is_ge, ALU.mult, ['cnt'], ['selw'])
                stt(lo, selw, w0_, lo, ALU.mult, ALU.add, ['selw', 'w0', 'lo'], ['lo'])
        else:
            memset('vector', lo, NEG * 0.5, ['lo'])
        ts('vector', mb[:, 0:L], score[:, 0:L], lo, MBV, ALU.is_lt, ALU.mult, ['score', 'lo'], mbk)

    def dsa_attention(ti, qbl):
        Q = ti * 4 + qbl
        qsl = slice(qbl * 128, (qbl + 1) * 128)
        mb = mbuf[Q % 2]
        mbk = MBK[Q % 2]
        qb0, qb0k = qk_next()
        qb1, qb1k = qk_next()
        for h in range(8):
            bank, bk = (qb0, qb0k) if h % 2 == 0 else (qb1, qb1k)
            hp = slice((h % 2) * 64, (h % 2 + 1) * 64)
            mm(bank[:, (h // 2) * 128:(h // 2 + 1) * 128], wuk_sb[hp, h // 2, :], A[hp, 2 + h // 2, qsl], True, True,
               ['wuk', ('A', 2 + h // 2)], [bk])
        qlv = qlb.rearrange("p (hp two t) -> p hp two t", two=2, t=128)
        act(qlv[:, :, 0, :], qb0.rearrange("p (h t) -> p h t", t=128), AF.Copy, [qb0k], QLK, scale=0.125)
        act(qlv[:, :, 1, :], qb1.rearrange("p (h t) -> p h t", t=128), AF.Copy, [qb1k], QLK, scale=0.125)
        accA = (pb[6], ('pb', 6)); accB = (pb[7], ('pb', 7)); accC = (pb[3], ('pb', 3))
        for st_ in range(Q + 1):
            ssl = slice(st_ * 128, (st_ + 1) * 128)
            ck = [('ckvT', st_ // 4)]
            PT = PTb[st_ % 2]
            PTK = PTKb[st_ % 2]
            lbs = [qk_next(), qk_next()]
            for half in range(2):
                lb_, lk_ = lbs[half]
                mm(lb_, ckvT[:, ssl], qlb[:, half * 512:(half + 1) * 512], True, False, ck + [QLK[half]], [lk_], sgc=True)
                for hh in range(4):
                    mm(lb_[:, hh * 128:(hh + 1) * 128], mb[:, ssl], ident, False, hh == 3, mbk + ['ident'], [lk_], sgc=True)
                act(PT[:, half * 512:(half + 1) * 512], lb_, AF.Exp, [lk_], [PTK[half]])
            ctk = [('ckvtok', st_ // 4)]
            first = (st_ == 0)
            last = (st_ == Q)
            mm(accA[0][:, 0:384], ckvtok[:, st_, :], PT[:, 0:384], first, last, ctk + PTK, [accA[1]])
            mm(accB[0][:, 0:384], ckvtok[:, st_, :], PT[:, 384:768], first, last, ctk + PTK, [accB[1]])
            mm(accC[0][:, 0:256], ckvtok[:, st_, :], PT[:, 768:1024], first, last, ctk + PTK, [accC[1]], sgc=True)
            for h in range(8):
                mm(accC[0][:, 256 + h:257 + h], PT[:, h * 128:(h + 1) * 128], ones[:, 0:1], False, last, PTK + ['ones'], [accC[1]], sgc=True)
        act(olat[:, 0:384], accA[0][:, 0:384], AF.Copy, [accA[1]], OLK)
        act(olat[:, 384:768], accB[0][:, 0:384], AF.Copy, [accB[1]], OLK)
        act(olat[:, 768:1024], accC[0][:, 0:256], AF.Copy, [accC[1]], OLK)
        P.emit('vector', lambda e, src_=accC[0][:, 256:264]: e.reciprocal(out=rden, in_=src_), [accC[1]], ['rden'])
        ob_, obk_ = qk_next()
        for h in range(8):
            mm(ob_[:, h * 64:(h + 1) * 64], olat[:, h * 128:(h + 1) * 128], wuv_sb[:, h, :], True, True, OLK + ['wuv'], [obk_])
        tt('vector', otok.rearrange("p (h d) -> p h d", d=64), ob_.rearrange("p (h d) -> p h d", d=64),
           rden.unsqueeze(2).to_broadcast([128, 8, 64]), ALU.mult, [obk_, 'rden'], [OTK])
        p, pk = qk_next()
        p = p.bitcast(BF16)
        for k_ in range(4):
            tr(p[:, k_ * 128:(k_ + 1) * 128], otok[:, k_ * 128:(k_ + 1) * 128], ident, [OTK, 'ident'], [pk])
        MK4 = [('A', 10 + c) for c in range(4)]
        cp('scalar', A[:, 10:14, qsl], p[:, 0:512].rearrange("p (k t) -> p k t", t=128), [pk], MK4)


    def layer1(ti):
        dry = wstate['dry']
        if not dry:
            norm_T(xs, 4, G_OMIX, hT, HTK, XK)
        w0, k0 = wnext('oin0')
        tsl_ = slice(ti * T, (ti + 1) * T)
        if not dry:
            sqb = [PTx[:, 0, :], PTx[:, 1, :], osq, A[:, 10, :]]
            sqk = [('PTx', 0), ('PTx', 1), 'osq', ('A', 10)]
            raw = [F(0), F(1), F(3), F(5)]
            cols = [0, 128, 256, 384]
            for i in range(4):
                bank, bk = proj_fm(w0, k0, cols[i], None)
                act(sqb[i], bank, AF.Square if i < 3 else AF.Copy, [bk], [sqk[i]])
                cp('vector', raw[i][0], bank, [bk], [raw[i][1]])
            pq, pqk = pb[6], ('pb', 6)
            pc, pck = pb[7], ('pb', 7)
            pi, pik = pb[3], ('pb', 3)
            for c in range(2):
                mm(pq, ones, sqb[c], c == 0, c == 1, ['ones', sqk[c]], [pqk])
            mm(pc, ones, sqb[2], True, True, ['ones', sqk[2]], [pck])
            mm(pi, ones, sqb[3], True, True, ['ones', sqk[3]], [pik])
            f2, f2k = F(2); f3, f3k = F(3); f4, f4k = F(4); f5, f5k = F(5); f6, f6k = F(6)
            act(f2, pq, AF.Sqrt, [pqk], [f2k], scale=1.0 / 256, bias=epsD)
            P.emit('vector', lambda e, f2=f2: e.reciprocal(out=f2, in_=f2), [f2k], [f2k])
            for c in range(2):
                f, fk = F(c)
                stt(A[:, c, :], f, cqg[:, c:c + 1], f2, ALU.mult, ALU.mult, [fk, 'cqg', f2k], [('A', c)])
            act(f4, pc, AF.Sqrt, [pck], [f4k], scale=1.0 / 128, bias=epsD)
            P.emit('vector', lambda e, f4=f4: e.reciprocal(out=f4, in_=f4), [f4k], [f4k])
            stt(ckvT[:, tsl_], f3, ckvg[:, 0:1], f4, ALU.mult, ALU.mult, [f3k, 'ckvg', f4k], [('ckvT', ti)])
            stt(f5, pi, -1.0 / 128, f5, ALU.mult, ALU.add, [pik, f5k], [f5k])
            act(A[:, 11, :], f5, AF.Square, [f5k], [('A', 11)])
            for j in range(4):
                bank, bk = pm_next()
                for kc in range(8):
                    mm(bank[:, 0:8], hT[:, kc, j * 128:(j + 1) * 128], widx_sb[:, kc, :], kc == 0, kc == 7, ['widx'] + HTK, [bk])
                cp('vector', wraw[:, j, :], bank[:, 0:8], [bk], [('wraw', j)])
            WRK = [('wraw', j) for j in range(4)]
            act(wabs, wraw, AF.Abs, WRK, ['wabs'], scale=(8.0 ** -0.5) * 0.125)
            act(wsgn, wraw, AF.Sign, WRK, ['wsgn'])
        w1, k1 = wnext('oin1')
        if not dry:
            for c in range(4):
                bank, bk = proj_fm(w1, k1, c * 128, None)
                act(A[:, 14 + c, :], bank, AF.Gelu, [bk], [('A', 14 + c)])
            mm(pi, ones, A[:, 11, :], True, True, ['ones', ('A', 11)], [pik])
            act(f6, pi, AF.Sqrt, [pik], [f6k], scale=1.0 / 128, bias=eps5)
            P.emit('vector', lambda e, f6=f6: e.reciprocal(out=f6, in_=f6), [f6k], [f6k])
            tt('vector', f5, f5, f6, ALU.mult, [f5k, f6k], [f5k])
            ts('vector', kidxT2[:, tsl_], f5, kig[:, 0:1], kib[:, 0:1], ALU.mult, ALU.add, [f5k, 'kig', 'kib'], [('kidx', ti)])
            p, pk = pt_next()
            for j in range(4):
                tr(p[:, j * 128:(j + 1) * 128], ckvT[:, ti * T + j * 128: ti * T + (j + 1) * 128], ident, [('ckvT', ti), 'ident'], [pk])
            cp('scalar', ckvtok[:, ti * 4:(ti + 1) * 4, :], p[:, 0:512].rearrange("p (j c) -> p j c", c=128), [pk], [('ckvtok', ti)])
            for oc in range(4):
                bank, bk = pm_next()
                for c in range(2):
                    mm(bank, wuq_sb[:, c, oc * 128:(oc + 1) * 128], A[:, c, :], c == 0, c == 1, ['wuq', ('A', c)], [bk])
                cp('scalar', A[:, 2 + oc, :], bank, [bk], [('A', 2 + oc)])
                bank, bk = pm_next()
                for c in range(2):
                    mm(bank, iwq_sb[:, c, oc * 128:(oc + 1) * 128], A[:, c, :], c == 0, c == 1, ['iwq', ('A', c)], [bk])
                cp('scalar', A[:, 6 + oc, :], bank, [bk], [('A', 6 + oc)])
        mark('l1_prep_done')
        w2_, k2 = wnext('oin2')
        if not dry and KL1 >= 2:
            for j in range(4):
                bank, bk = pm_next()
                for kc in range(8):
                    mm(bank, hT[:, kc, j * 128:(j + 1) * 128], w2_[:, kc, :], kc == 0, kc == 7, [k2] + HTK, [bk])
                f, fk = F(j % 2)
                g, gk = F(2 + j % 2)
                act(f, bank, AF.Gelu, [bk], [fk, ('lnst', j)], accum=lnst[:, j, 0:1])
                ts('vector', lnst[:, j, 1:2], lnst[:, j, 0:1], -1.0 / 512, None, ALU.mult, None, [('lnst', j)], [('lnst', j)])
                ts('vector', f, f, lnst[:, j, 1:2], None, ALU.add, None, [fk, ('lnst', j)], [fk])
                act(g, f, AF.Square, [fk], [gk, ('lnst', j)], accum=lnst[:, j, 2:3])
                act(lnst[:, j, 3:4], lnst[:, j, 2:3], AF.Sqrt, [('lnst', j)], [('lnst', j)], scale=1.0 / 512, bias=eps5)
                P.emit('vector', lambda e, j=j: e.reciprocal(out=lnst[:, j, 3:4], in_=lnst[:, j, 3:4]), [('lnst', j)], [('lnst', j)])
                stt(g, f, lnst[:, j, 3:4], vg_bc, ALU.mult, ALU.mult, [fk, ('lnst', j), 'vgbc'], [gk])
                tt('vector', KTt[:, j, :], g, vb_bc, ALU.add, [gk, 'vbbc'], [('KTt', j)])
                bank, bk = pm_next()
                for g_ in range(4):
                    mm(bank[:, g_ * 128:(g_ + 1) * 128], KTt[:, j, g_ * 128:(g_ + 1) * 128], wT_sb[:, g_, :], True, True, [('KTt', j), 'wT'], [bk])
                tt('vector', f, bank, bs_bc.rearrange("p g t -> p (g t)"), ALU.add, [bk, 'bsbc'], [fk])
                av = A[:, 14:18, j * 128:(j + 1) * 128]
                AK = [('A', 14 + c) for c in range(4)]
                tt('vector', av, f.rearrange("p (g t) -> p g t", t=128), av, ALU.mult, [fk] + AK, AK)
            mark('l1_gmlp_done')
            order = ['i0', 't0', 'i1', 't1', 'a0', 'i2', 't2', 'a1', 'i3', 't3', 'a2', 'a3']
            if os.environ.get('KSEQ', '0') == '1':
                order = ['i0', 't0', 'a0', 'i1', 't1', 'a1', 'i2', 't2', 'a2', 'i3', 't3', 'a3']
            for step in order:
                qbl = int(step[1])
                if KL1 < 3 or (KL1 == 3 and step[0] != 'i') or (KL1 == 4 and step[0] == 'a'):
                    continue
                if step[0] == 'i':
                    dsa_indexer(ti, qbl)
                elif step[0] == 't':
                    dsa_threshold(ti, qbl)
                else:
                    dsa_attention(ti, qbl)
        mark('l1_dsa_done')
        if os.environ.get('KSTOP', '0') == '1':
            return
        resid_add(lambda nb, kg: f'oout{nb}', A[:, 10:18, :], [('A', 10 + c) for c in range(8)], [8])

    gfin = Fb[:, 0:2, :].rearrange("p a t -> p (a t)")

    wstate['dry'] = True
    wstate['pos'] = 0
    for ti in range(NT):
        tile_body(ti)
    wstate['dry'] = False
    wstate['pos'] = 0
    dmy = sb("dmy", [128, 4], F32)
    P.barrier({
        'vector': (lambda e: e.memset(dmy[:, 0:1], 0.0), ['dmy0']),
        'gpsimd': (lambda e: e.memset(dmy[:, 1:2], 0.0), ['dmy1']),
        'scalar': (lambda e: e.activation(out=dmy[:, 2:3], in_=epsD, func=AF.Copy), ['dmy2']),
        'tensor': (lambda e: e.matmul(pb[0][0:1, 0:1], lhsT=ones[:, 0:1], rhs=ones[:, 0:1], start=True, stop=True), [('pb', 0)]),
    })
    emit_casts([f'wkv{l}_{nb}' for l in range(nlayers) for nb in range(4)])
    emit_casts(list(wseq))
    pending.clear()
    for l in range(nlayers):
        mem_kv(l)
    for ti in range(NT):
        tile_body(ti)
    P.finalize()
    return nc


_CACHE = {}
MARKS = []


def kernel(**inputs):
    x = np.ascontiguousarray(inputs['x'], dtype=np.float32)
    mem = np.ascontiguousarray(inputs['mem'], dtype=np.float32)
    B, S, _ = x.shape
    if S not in _CACHE:
        _CACHE[S] = build(S)
    nc = _CACHE[S]
    in_maps = []
    for b in range(B):
        m = {"x": x[b], "mem": mem[b]}
        for name, shp in WEIGHT_SPECS:
            m[name] = np.ascontiguousarray(inputs[name], dtype=np.float32)
        in_maps.append(m)
    res = run_bass_kernel_spmd(nc, in_maps, core_ids=list(range(B)))
    return np.stack([r["y"] for r in res.results], axis=0)
```

```python
import contextlib
import numpy as np
import concourse.bass as bass
import concourse.mybir as mybir
from concourse.bass_utils import run_bass_kernel_spmd

F32 = mybir.dt.float32
BF16 = mybir.dt.bfloat16
AF = mybir.ActivationFunctionType
ALU = mybir.AluOpType
AX = mybir.AxisListType

EPOCH = 30000
NSLOT = 16
D = 1024
T = 512
FF = 2816
NEG = -1.0e30
import os
STAGE = int(os.environ.get('KSTAGE', '9'))
SUB = int(os.environ.get('KSUB', '9'))
KL1 = int(os.environ.get('KL1', '9'))


class Prog:
    ENGS = ['tensor', 'vector', 'scalar', 'gpsimd', 'sync']

    def __init__(self, nc):
        self.nc = nc
        self.q = {e: [] for e in self.ENGS}
        self.cnt = {e: 0 for e in self.ENGS}
        self.dcnt = {e: 0 for e in self.ENGS}
        self.known = {e: {} for e in self.ENGS}
        self.last_w = {}
        self.readers = {}

    def _need(self, waits, ev, eng, raw):
        if ev is None:
            return
        kind, s, n = ev
        if kind == 'E' and s == eng:
            if eng == 'tensor':
                return
        k = (kind, s)
        if self.known[eng].get(k, 0) >= n:
            return
        if waits.get(k, 0) < n:
            waits[k] = n

    def emit(self, eng, fn, reads=(), writes=(), dma=False, extra=()):
        pr = [k for k in reads if isinstance(k, tuple) and k[0] == 'pb']
        if pr:
            reads = [k for k in reads if not (isinstance(k, tuple) and k[0] == 'pb')]
            writes = list(writes) + [k for k in pr if k not in writes]
        waits = {}
        for ev in extra:
            self._need(waits, ev, eng, True)
        for k in reads:
            self._need(waits, self.last_w.get(k), eng, True)
        for k in writes:
            self._need(waits, self.last_w.get(k), eng, False)
            for kk, n in self.readers.get(k, {}).items():
                self._need(waits, (kk[0], kk[1], n), eng, False)
        if dma:
            i = self.dcnt[eng]
            self.dcnt[eng] = i + 1
            slot = i % NSLOT
            n = i // NSLOT + 1
            if n > 1:
                self._need(waits, ('D', (eng, slot), n - 1), eng, False)
            ev = ('D', (eng, slot), n)
        else:
            n = self.cnt[eng] + 1
            self.cnt[eng] = n
            ev = ('E', eng, n)
        for k, n_ in waits.items():
            self.known[eng][k] = n_
        kk = (ev[0], ev[1])
        for k in reads:
            d = self.readers.setdefault(k, {})
            if d.get(kk, 0) < ev[2]:
                d[kk] = ev[2]
        for k in writes:
            self.last_w[k] = ev
            self.readers[k] = {}
        self.q[eng].append((list(waits.items()), fn, ev))
        return ev

    def all_events(self):
        evs = []
        for e in self.ENGS:
            if self.cnt[e] > 0:
                evs.append(('E', e, self.cnt[e]))
            for slot in range(min(NSLOT, self.dcnt[e])):
                evs.append(('D', (e, slot), (self.dcnt[e] - 1 - slot) // NSLOT + 1))
        return evs

    def barrier(self, dummies):
        evs = self.all_events()
        for eng, (fn, writes) in dummies.items():
            self.emit(eng, fn, (), writes, extra=evs)

    def finalize(self):
        nc = self.nc
        st = contextlib.ExitStack()
        esem = {}
        for e in self.ENGS:
            ne = self.cnt[e] // EPOCH + 1
            esem[e] = [st.enter_context(nc.semaphore(f"s_{e}_{j}")) for j in range(ne)]
        dsem = {}
        per = EPOCH // 16
        for e in self.ENGS:
            for slot in range(min(NSLOT, self.dcnt[e])):
                nd = (self.dcnt[e] + NSLOT - 1) // NSLOT
                ne = nd // per + 1
                dsem[(e, slot)] = [st.enter_context(nc.semaphore(f"d_{e}_{slot}_{j}")) for j in range(ne)]

        def hw(kind, s, n):
            if kind == 'E':
                return esem[s][(n - 1) // EPOCH], (n - 1) % EPOCH + 1
            return dsem[s][(n - 1) // per], ((n - 1) % per + 1) * 16

        with nc.Block() as block:
            def run(engname, engobj):
                for waits, fn, ev in self.q[engname]:
                    for (kind, s), n in waits:
                        sem, val = hw(kind, s, n)
                        engobj.wait_ge(sem, val)
                    inst = fn(engobj)
                    sem, val = hw(*ev)
                    inst.then_inc(sem, 16 if ev[0] == 'D' else 1)
                if engname == 'sync':
                    for e in self.ENGS:
                        if self.cnt[e] > 0:
                            sem, val = hw('E', e, self.cnt[e])
                            engobj.wait_ge(sem, val)
                        for slot in range(min(NSLOT, self.dcnt[e])):
                            nlast = (self.dcnt[e] - 1 - slot) // NSLOT + 1
                            sem, val = hw('D', (e, slot), nlast)
                            engobj.wait_ge(sem, val)

            @block.tensor
            def _(eng):
                run('tensor', eng)

            @block.vector
            def _(eng):
                run('vector', eng)

            @block.scalar
            def _(eng):
                run('scalar', eng)

            @block.gpsimd
            def _(eng):
                run('gpsimd', eng)

            @block.sync
            def _(eng):
                run('sync', eng)
        st.close()


WEIGHT_SPECS = [
    ('hgrn_lower_bounds', [3, 512]), ('even_mix_norm', [1, 1024]), ('even_w_in', [1, 1024, 3584]),
    ('even_a_out_norm', [1, 4, 128]), ('even_b_conv', [1, 3, 512]), ('even_w_out', [1, 1024, 1024]),
    ('odd_mix_norm', [1, 1024]), ('odd_w_in', [1, 1024, 1480]), ('odd_c_q_norm', [1, 256]),
    ('odd_c_kv_norm', [1, 128]), ('odd_c_w_uq', [1, 256, 512]), ('odd_c_w_uk', [1, 8, 64, 128]),
    ('odd_c_w_uv', [1, 8, 128, 64]), ('odd_c_idx_wq', [1, 256, 512]), ('odd_c_idx_k_g', [1, 64]),
    ('odd_c_idx_k_b', [1, 64]), ('odd_d_v_g', [1, 512]), ('odd_d_v_b', [1, 512]),
    ('odd_d_w_s', [1, 4, 128, 128]), ('odd_d_b_s', [1, 4, 128]), ('odd_w_out', [1, 1024, 1024]),
    ('xa_norm', [2, 1024]), ('xa_mem_norm', [2, 1024]), ('xa_wq', [2, 1024, 1024]),
    ('xa_wkv', [2, 1024, 2048]), ('xa_wo', [2, 1024, 1024]), ('ffn_norm', [2, 1024]),
    ('ffn_w13', [2, 1024, 5632]), ('ffn_w2', [2, 2816, 1024]), ('final_norm', [1024]),
]


def build(S, nlayers=2):
    NT = S // T
    nc = bass.Bass("TRN2", target_bir_lowering=False)
    P = Prog(nc)
    x_d = nc.dram_tensor("x", [S, D], F32, kind="ExternalInput").ap()
    mem_d = nc.dram_tensor("mem", [256, D], F32, kind="ExternalInput").ap()
    y_d = nc.dram_tensor("y", [S, D], F32, kind="ExternalOutput").ap()
    Wd = {}
    for name, shp in WEIGHT_SPECS:
        Wd[name] = nc.dram_tensor(name, shp, F32, kind="ExternalInput").ap()

    def sb(name, shape, dt):
        return nc.alloc_sbuf_tensor(name, shape, dt).ap()

    def mm(out, lhsT, rhs, start, stop, reads, writes, sgc=False):
        P.emit('tensor', lambda e: e.matmul(out, lhsT=lhsT, rhs=rhs, start=start, stop=stop, skip_group_check=sgc), reads, writes)

    def tr(out, in_, ident, reads, writes):
        P.emit('tensor', lambda e: e.transpose(out=out, in_=in_, identity=ident), reads, writes)

    def act(out, in_, func, reads, writes, scale=None, bias=None, accum=None):
        kw = {}
        if scale is not None:
            kw['scale'] = scale
        if bias is not None:
            kw['bias'] = bias
        if accum is not None:
            kw['accum_out'] = accum
        P.emit('scalar', lambda e: e.activation(out=out, in_=in_, func=func, **kw), reads, writes)

    def ts(eng, out, in0, s1, s2, op0, op1, reads, writes, accum=None):
        kw = {}
        if accum is not None:
            kw['accum_out'] = accum
        if out.dtype == mybir.dt.float8e4:
            kw['saturate'] = False
        if op1 is None:
            P.emit(eng, lambda e: e.tensor_scalar(out=out, in0=in0, scalar1=s1, scalar2=None, op0=op0, **kw), reads, writes)
        else:
            P.emit(eng, lambda e: e.tensor_scalar(out=out, in0=in0, scalar1=s1, scalar2=s2, op0=op0, op1=op1, **kw), reads, writes)

    def tt(eng, out, in0, in1, op, reads, writes):
        P.emit(eng, lambda e: e.tensor_tensor(out=out, in0=in0, in1=in1, op=op), reads, writes)

    def stt(out, in0, scalar, in1, op0, op1, reads, writes):
        P.emit('vector', lambda e: e.scalar_tensor_tensor(out=out, in0=in0, scalar=scalar, in1=in1, op0=op0, op1=op1), reads, writes)

    def cp(eng, out, in_, reads, writes):
        if eng == 'scalar':
            P.emit(eng, lambda e: e.copy(out=out, in_=in_), reads, writes)
        else:
            P.emit(eng, lambda e: e.tensor_copy(out=out, in_=in_), reads, writes)

    def memset(eng, ap, val, writes):
        P.emit(eng, lambda e: e.memset(ap, val), (), writes)

    def dma(eng, out, in_, reads, writes, slow=False):
        if slow:
            P.emit(eng, lambda e: e.dma_start(out=out, in_=in_, allow_slow_non_contiguous=True), reads, writes, dma=True)
        else:
            P.emit(eng, lambda e: e.dma_start(out=out, in_=in_), reads, writes, dma=True)

    wblocks = {}
    cast_i = [0]

    def make_block(key, pieces, kcn, ncols):
        wb = nc.dram_tensor("wb_" + key, [128, kcn, ncols], BF16).ap()
        pending[key] = (wb, pieces)
        wblocks[key] = (wb, kcn, ncols)

    pending = {}

    def emit_casts(order):
        for key in order:
            if key not in pending:
                continue
            wb, pieces = pending.pop(key)
            for src, c0 in pieces:
                n = src.shape[1]
                dma('gpsimd', wb[:, :, c0:c0 + n], src.rearrange("(kc p) n -> p kc n", p=128), (), [('wb', key)])

    for nb in range(7):
        make_block(f'ein{nb}', [(Wd['even_w_in'][0][:, nb * 512:(nb + 1) * 512], 0)], 8, 512)
    for nb in range(2):
        make_block(f'eout{nb}', [(Wd['even_w_out'][0][:, nb * 512:(nb + 1) * 512], 0)], 8, 512)
    for l in range(2):
        for nb in range(2):
            make_block(f'wq{l}_{nb}', [(Wd['xa_wq'][l][:, nb * 512:(nb + 1) * 512], 0)], 8, 512)
            make_block(f'wo{l}_{nb}', [(Wd['xa_wo'][l][:, nb * 512:(nb + 1) * 512], 0)], 8, 512)
        for nb in range(4):
            make_block(f'wkv{l}_{nb}', [(Wd['xa_wkv'][l][:, nb * 512:(nb + 1) * 512], 0)], 8, 512)
        for p_ in range(11):
            make_block(f'w13_{l}_{p_}', [(Wd['ffn_w13'][l][:, p_ * 256:(p_ + 1) * 256], 0),
                                         (Wd['ffn_w13'][l][:, FF + p_ * 256:FF + (p_ + 1) * 256], 256)], 8, 512)
        for half, (c0, groups) in enumerate([(0, [8, 2]), (10, [8, 4])]):
            for nb in range(2):
                k0 = c0
                for kg, kn in enumerate(groups):
                    make_block(f'w2_{l}_{half}_{nb}_{kg}', [(Wd['ffn_w2'][l][k0 * 128:(k0 + kn) * 128, nb * 512:(nb + 1) * 512], 0)], kn, 512)
                    k0 += kn
    if nlayers > 1:
        make_block('oin0', [(Wd['odd_w_in'][0][:, 0:448], 0), (Wd['odd_w_in'][0][:, 384:448], 448)], 8, 512)
        make_block('oin1', [(Wd['odd_w_in'][0][:, 456:968], 0)], 8, 512)
        make_block('oin2', [(Wd['odd_w_in'][0][:, 968:1480], 0)], 8, 512)
        for nb in range(2):
            make_block(f'oout{nb}', [(Wd['odd_w_out'][0][:, nb * 512:(nb + 1) * 512], 0)], 8, 512)

    ident = sb("ident", [128, 128], BF16)
    ones = sb("ones", [128, 128], BF16)
    xs = sb("xs", [128, 4, D], F32)
    xn = sb("xn", [128, 1, D], BF16)
    hT = sb("hT", [128, 8, T], BF16)
    A = sb("A", [128, 18, T], BF16)
    Fb = sb("F", [128, 8, T], F32)
    Vt = sb("Vt", [128, 4, T], BF16)
    KTt = sb("KTt", [128, 4, T], BF16)
    WR = sb("WR", [128, 4, 8, 512], BF16)
    gall = sb("gall", [128, 7, 8], F32)
    ss = sb("ss", [128, 4], F32)
    rs = sb("rs", [128, 4], F32)
    xaK = sb("xaK", [128, 2, 8, 256], BF16)
    xaV = sb("xaV", [128, 2, 2, D], BF16)
    Sst = sb("Sst", [128, 4, 128], F32)
    Sbf = sb("Sbf", [128, 4, 128], BF16)
    lbr = sb("lbr", [128, 4, 3], F32)
    lbs = sb("lbs", [128, 4], F32)
    lb = sb("lb", [128, 4], F32)
    oml = sb("oml", [128, 4], F32)
    again = sb("again", [128, 4], F32)
    convw = sb("convw", [128, 4, 3], F32)
    zc = sb("zc", [128, 4, T + 2], BF16)
    EBL = sb("EBL", [128, 4, 8], F32)
    mpair = sb("mpair", [128, 128], F32)
    rmask = sb("rmask", [128, T], BF16)
    attn_sb = sb("attn_sb", [128, 2, 128], BF16)
    osq = sb("osq", [128, T], BF16)
    PTx = sb("PTx", [128, 2, T], BF16)
    junk = PTx.rearrange("p a t -> p (a t)")
    JK = [('PTx', 0), ('PTx', 1)]

    pb = [nc.alloc_psum_tensor(f"pb{i}", [128, 512], F32).ap() for i in range(8)]
    pm_i = [0]

    PMB = (0, 1, 2)

    def pm_next():
        i = PMB[pm_i[0] % len(PMB)]
        pm_i[0] += 1
        return pb[i], ('pb', i)

    def pm4():
        return [(pb[i], ('pb', i)) for i in range(4)]

    pt_i = [0]

    def pt_next():
        i = 4 + pt_i[0] % 2
        pt_i[0] += 1
        return pb[i].bitcast(BF16), ('pb', i)

    memset('gpsimd', ident, 0.0, ['ident'])
    P.emit('gpsimd', lambda e: e.affine_select(out=ident, in_=ident, pattern=[[-1, 128]], compare_op=ALU.not_equal,
                                               fill=1.0, base=0, channel_multiplier=1), ['ident'], ['ident'])
    memset('vector', ones, 1.0, ['ones'])
    memset('vector', mpair, 1.0, ['mpair'])
    P.emit('gpsimd', lambda e: e.affine_select(out=mpair, in_=mpair, pattern=[[1, 128]], compare_op=ALU.is_ge,
                                               fill=0.0, base=0, channel_multiplier=-1), ['mpair'], ['mpair'])
    memset('gpsimd', mpair[0:64, 64:128], 0.0, ['mpair'])
    memset('vector', rmask, 1.0, ['rmask'])
    memset('vector', rmask.rearrange("p (c s) -> p c s", s=64)[:, :, 0:1], 0.0, ['rmask'])
    memset('vector', zc, 0.0, [('zc', c) for c in range(4)])
    memset('vector', Sst, 0.0, [('S', h) for h in range(4)])
    memset('vector', Sbf, 0.0, [('Sbf', h) for h in range(4)])

    gsrc = [Wd['even_mix_norm'][0], Wd['xa_norm'][0], Wd['ffn_norm'][0], Wd['odd_mix_norm'][0],
            Wd['xa_norm'][1], Wd['ffn_norm'][1], Wd['final_norm']]
    for i, g in enumerate(gsrc):
        dma('sync', gall[:, i, :], g.rearrange("(kc p) -> p kc", p=128), (), ['gall'], slow=True)
    G_EMIX, G_XA0, G_FFN0, G_OMIX, G_XA1, G_FFN1, G_FIN = range(7)
    for h in range(4):
        dma('sync', lbr[:, h, :], Wd['hgrn_lower_bounds'][:, h * 128:(h + 1) * 128].rearrange("s k -> k s"), (), ['lbr'], slow=True)
    dma('sync', again, Wd['even_a_out_norm'][0].rearrange("h v -> v h"), (), ['again'], slow=True)
    for c in range(4):
        dma('sync', convw[:, c, :], Wd['even_b_conv'][0][:, c * 128:(c + 1) * 128].rearrange("k p -> p k"), (), ['convw'], slow=True)
    act(lbr, lbr, AF.Exp, ['lbr'], ['lbr'])
    P.emit('vector', lambda e: e.reduce_sum(out=lbs, in_=lbr, axis=AX.X), ['lbr'], ['lbs'])
    P.emit('vector', lambda e: e.reciprocal(out=lbs, in_=lbs), ['lbs'], ['lbs'])
    tt('vector', lb, lbr[:, :, 0], lbs, ALU.mult, ['lbr', 'lbs'], ['lb'])
    ts('vector', oml, lb, -1.0, 1.0, ALU.mult, ALU.add, ['lb'], ['oml'])

    wseq = []
    wstate = {'pos': 0, 'loaded': 0, 'dry': True}

    def wload(m):
        key = wseq[m]
        wb, kcn, ncols = wblocks[key]
        slot = m % 4
        dma('sync', WR[:, slot, 0:kcn, 0:ncols], wb, [('wb', key)], [('W', slot)])

    def wnext(key, keep_prev=False):
        i = wstate['pos']
        wstate['pos'] = i + 1
        if wstate['dry']:
            wseq.append(key)
            return None, None
        assert wseq[i] == key, (i, wseq[i], key)
        lim = min(len(wseq) - 1, i + (1 if keep_prev else 2))
        while wstate['loaded'] <= lim:
            wload(wstate['loaded'])
            wstate['loaded'] += 1
        slot = i % 4
        return WR[:, slot], ('W', slot)

    def norm_T(src, nsub, gidx, dst, dkeys, skeys):
        for j in range(nsub):
            act(junk, src[:, j, :], AF.Square, [skeys[j]], JK + [('ss', j)], accum=ss[:, j:j + 1])
        act(rs[:, 0:nsub], ss[:, 0:nsub], AF.Sqrt, [('ss', j) for j in range(nsub)], ['rs'], scale=1.0 / D, bias=epsD)
        P.emit('vector', lambda e: e.reciprocal(out=rs[:, 0:nsub], in_=rs[:, 0:nsub]), ['rs'], ['rs'])
        for j in range(nsub):
            act(xn[:, 0, :], src[:, j, :], AF.Copy, [skeys[j], 'rs'], [('xn', 0)], scale=rs[:, j:j + 1])
            p, pk = pt_next()
            for kc in range(8):
                tr(p[:, kc * 128:(kc + 1) * 128], xn[:, 0, kc * 128:(kc + 1) * 128], ident, [('xn', 0), 'ident'], [pk])
            tt('vector', dst[:, :, j * 128:(j + 1) * 128], p.rearrange("p (k t) -> p k t", t=128),
               gall[:, gidx, :].unsqueeze(2).to_broadcast([128, 8, 128]), ALU.mult, [pk, 'gall'], dkeys)

    HTK = [('hT', kc) for kc in range(8)]
    XK = [('x', j) for j in range(4)]

    def proj_fm(wv, wk, col0, handler_bank):
        bank, bk = pm_next()
        for kc in range(8):
            mm(bank, wv[:, kc, col0:col0 + 128], hT[:, kc, :], kc == 0, kc == 7, [wk] + HTK, [bk])
        return bank, bk

    def resid_add(wkeys_fn, inT, inkeys, nkc_groups):
        for nb in range(2):
            banks = pm4()
            kc0 = 0
            ng = len(nkc_groups)
            for kg in range(ng):
                wv, wk = wnext(wkeys_fn(nb, kg))
                if wstate['dry']:
                    continue
                kn = nkc_groups[kg]
                for j in range(4):
                    bank, bk = banks[j]
                    for kc in range(kn):
                        mm(bank, inT[:, kc0 + kc, j * 128:(j + 1) * 128], wv[:, kc, :], kc0 + kc == 0,
                           (kg == ng - 1 and kc == kn - 1), [wk] + inkeys, [bk])
                kc0 += kn
            if wstate['dry']:
                continue
            for j in range(4):
                bank, bk = banks[j]
                tt('vector', xs[:, j, nb * 512:(nb + 1) * 512], xs[:, j, nb * 512:(nb + 1) * 512], bank, ALU.add,
                   [bk, ('x', j)], [('x', j)])

    def F(i):
        return Fb[:, i, :], ('F', i)

    epsD = sb("epsD", [128, 1], F32)
    memset('vector', epsD, 1e-6, ['eps'])

    def conv_branch(mix):
        dry = wstate['dry']
        zh = lambda c: (A[:, 8 + c, :], ('A', 8 + c))
        w_bh, k_bh = wnext('ein6')
        if not dry:
            for c in range(4):
                bank, bk = proj_fm(w_bh, k_bh, c * 128, None)
                zv, zk = zh(c)
                cp('scalar', zv, bank, [bk], [zk])
        w_bc, k_bc = wnext('ein5')
        if not dry:
            for c in range(4):
                bank, bk = proj_fm(w_bc, k_bc, c * 128, None)
                zv, zk = zh(c)
                tt('vector', zc[:, c, 2:T + 2], bank, zv, ALU.mult, [bk, zk], [('zc', c)])
        w_bb, k_bb = wnext('ein4')
        if not dry:
            for c in range(4):
                bank, bk = proj_fm(w_bb, k_bb, c * 128, None)
                f3, f3k = F(3); f4, f4k = F(4)
                ts('vector', f3, zc[:, c, 0:T], convw[:, c, 0:1], None, ALU.mult, None, [('zc', c), 'convw'], [f3k])
                stt(f4, zc[:, c, 1:T + 1], convw[:, c, 1:2], f3, ALU.mult, ALU.add, [('zc', c), 'convw', f3k], [f4k])
                stt(f3, zc[:, c, 2:T + 2], convw[:, c, 2:3], f4, ALU.mult, ALU.add, [('zc', c), 'convw', f4k], [f3k])
                mv, mk = mix(4 + c)
                tt('vector', mv, bank, f3, ALU.mult, [bk, f3k], [mk])
                cp('gpsimd', zc[:, c, 0:2], zc[:, c, T:T + 2], [('zc', c)], [('zc', c)])

    def xattn(l, gidx):
        dry = wstate['dry']
        if not dry:
            norm_T(xs, 4, gidx, hT, HTK, XK)
        qT = lambda c: (A[:, c, :], ('A', c))
        oT = lambda c: (A[:, 8 + c, :], ('A', 8 + c))
        for nb in range(2):
            wv, wk = wnext(f'wq{l}_{nb}')
            if dry:
                continue
            for c in range(4):
                bank, bk = proj_fm(wv, wk, c * 128, None)
                qv, qk = qT(nb * 4 + c)
                cp('scalar', qv, bank, [bk], [qk])
        if not dry:
            PTs = [(PTx, [('PTx', 0), ('PTx', 1)]), (A[:, 16:18, :], [('A', 16), ('A', 17)])]

            def xa_a(hd):
                PTv, PTk = PTs[hd % 2]
                for mj in range(2):
                    bank, bk = pm_next()
                    for dc in range(2):
                        qv, qk = qT(hd * 2 + dc)
                        mm(bank, xaK[:, l, hd * 2 + dc, mj * 128:(mj + 1) * 128], qv, dc == 0, dc == 1, [('xaK', l), qk], [bk])
                    act(PTv[:, mj, :], bank, AF.Exp, [bk], [PTk[mj]], scale=1.0 / 16.0)

            def xa_b(hd):
                PTv, PTk = PTs[hd % 2]
                pa, pak = pb[6 + hd % 2], ('pb', 6 + hd % 2)
                for mj in range(2):
                    mm(pa, ones, PTv[:, mj, :], mj == 0, mj == 1, ['ones', PTk[mj]], [pak])
                f2, f2k = F(2 + hd % 2)
                P.emit('vector', lambda e, f2=f2, pa=pa: e.reciprocal(out=f2, in_=pa), [pak], [f2k])
                for dc in range(2):
                    bank, bk = pm_next()
                    for mj in range(2):
                        mm(bank, xaV[:, l, mj, hd * 256 + dc * 128: hd * 256 + (dc + 1) * 128], PTv[:, mj, :], mj == 0, mj == 1,
                           [('xaV', l), PTk[mj]], [bk])
                    ov, ok = oT(hd * 2 + dc)
                    tt('vector', ov, bank, f2, ALU.mult, [bk, f2k], [ok])

            xa_a(0); xa_a(1); xa_b(0); xa_a(2); xa_b(1); xa_a(3); xa_b(2); xa_b(3)
        resid_add(lambda nb, kg: f'wo{l}_{nb}', A[:, 8:16, :], [('A', 8 + c) for c in range(8)], [8])

    def ffn(l, gidx):
        dry = wstate['dry']
        if not dry:
            norm_T(xs, 4, gidx, hT, HTK, XK)
        for half, (p0, p1, groups) in enumerate([(0, 5, [8, 2]), (5, 11, [8, 4])]):
            for p_ in range(p0, p1):
                wv, wk = wnext(f'w13_{l}_{p_}')
                if dry:
                    continue
                for cc in range(2):
                    bg, bgk = proj_fm(wv, wk, cc * 128, None)
                    bu, buk = proj_fm(wv, wk, 256 + cc * 128, None)
                    f, fk = F(4 + (p_ * 2 + cc) % 4)
                    act(f, bg, AF.Silu, [bgk], [fk])
                    c = (p_ - p0) * 2 + cc
                    tt('vector', A[:, c, :], bu, f, ALU.mult, [buk, fk], [('A', c)])
            nch = sum(groups)
            resid_add(lambda nb, kg, half=half: f'w2_{l}_{half}_{nb}_{kg}', A[:, 0:nch, :], [('A', c) for c in range(nch)], groups)

    def mem_kv(l):
        ms = xs
        dma('sync', ms[:, 0:2, :], mem_d.rearrange("(j p) d -> p j d", p=128), (), [('x', 0), ('x', 1)])
        dma('sync', gall[:, 6, :], Wd['xa_mem_norm'][l].rearrange("(kc p) -> p kc", p=128), (), ['gall'], slow=True)
        norm_T(ms, 2, 6, hT, HTK, XK)
        for nb in range(4):
            wb, kcn, ncols = wblocks[f'wkv{l}_{nb}']
            dma('sync', WR[:, nb % 4, :, :], wb, [('wb', f'wkv{l}_{nb}')], [('W', nb % 4)])
        for oc in range(8):
            bank, bk = pm_next()
            for kc in range(8):
                mm(bank[:, 0:256], WR[:, oc // 4, kc, (oc % 4) * 128:(oc % 4 + 1) * 128], hT[:, kc, 0:256], kc == 0, kc == 7,
                   [('W', oc // 4)] + HTK, [bk])
            cp('scalar', xaK[:, l, oc, :], bank[:, 0:256], [bk], [('xaK', l)])
        for mj in range(2):
            for nb in range(2):
                bank, bk = pm_next()
                for kc in range(8):
                    mm(bank, hT[:, kc, mj * 128:(mj + 1) * 128], WR[:, 2 + nb, kc, :], kc == 0, kc == 7, [('W', 2 + nb)] + HTK, [bk])
                cp('scalar', xaV[:, l, mj, nb * 512:(nb + 1) * 512], bank, [bk], [('xaV', l)])

    def mark(label):
        if not wstate['dry']:
            MARKS.append((label, P.cnt['tensor'], P.cnt['vector'], P.cnt['scalar']))

    def tile_body(ti):
        dry = wstate['dry']
        mark('tile_start')
        mixf = lambda c: (A[:, c, :], ('A', c))
        if not dry:
            dma('sync', xs, x_d[ti * T:(ti + 1) * T, :].rearrange("(j p) d -> p j d", p=128), (), XK)
            norm_T(xs, 4, G_EMIX, hT, HTK, XK)
        mark('l0_norm_done')
        if STAGE >= 1:
            layer0_mixer_main()
        mark('l0_hgrn_done')
        if STAGE >= 2:
            conv_branch(mixf)
            resid_add(lambda nb, kg: f'eout{nb}', A[:, 0:8, :], [('A', c) for c in range(8)], [8])
        mark('l0_conv_eout_done')
        if STAGE >= 3:
            xattn(0, G_XA0)
        mark('l0_xa_done')
        if STAGE >= 4:
            ffn(0, G_FFN0)
        mark('l0_ffn_done')
        if nlayers > 1:
            layer1(ti)
            mark('l1_mixer_done')
            if os.environ.get('KSTOP', '0') == '1':
                return
            xattn(1, G_XA1)
            mark('l1_xa_done')
            ffn(1, G_FFN1)
            mark('l1_ffn_done')
        if not dry:
            for j in range(4):
                act(junk, xs[:, j, :], AF.Square, [('x', j)], JK + [('ss', j)], accum=ss[:, j:j + 1])
            act(rs, ss, AF.Sqrt, [('ss', j) for j in range(4)], ['rs'], scale=1.0 / D, bias=epsD)
            P.emit('vector', lambda e: e.reciprocal(out=rs, in_=rs), ['rs'], ['rs'])
            dma('sync', gfin, Wd['final_norm'].partition_broadcast(128), (), [('F', 0), ('F', 1)])
            for j in range(4):
                stt(xs[:, j, :], xs[:, j, :], rs[:, j:j + 1], gfin, ALU.mult, ALU.mult, [('x', j), 'rs', ('F', 0), ('F', 1)], [('x', j)])
            dma('sync', y_d[ti * T:(ti + 1) * T, :].rearrange("(j p) d -> p j d", p=128), xs, XK, [('y', ti)])

    def layer0_mixer_main():
        dry = wstate['dry']
        _l0()

    def _l0():
        dry = wstate['dry']
        qd = lambda h: (A[:, 0 + h, :], ('A', 0 + h))
        kd = lambda h: (A[:, 4 + h, :], ('A', 4 + h))
        kdl = lambda h: (A[:, 8 + h, :], ('A', 8 + h))
        og = lambda h: (A[:, 12 + h, :], ('A', 12 + h))
        mix = lambda c: (A[:, c, :], ('A', c))
        w_aq, k_aq = wnext('ein0')
        w_af, k_af = wnext('ein1', keep_prev=True)
        if not dry:
            for h in range(4):
                bank, bk = proj_fm(w_af, k_af, h * 128, None)
                f0, f0k = F(0); f1, f1k = F(1); f2, f2k = F(2); f3, f3k = F(3)
                f4, f4k = F(4); f5, f5k = F(5); f6, f6k = F(6)
                act(f0, bank, AF.Sigmoid, [bk], [f0k])
                ts('vector', f1, f0, oml[:, h:h + 1], lb[:, h:h + 1], ALU.mult, ALU.add, [f0k, 'oml', 'lb'], [f1k])
                act(f2, f1, AF.Ln, [f1k], [f2k])
                ts('vector', f3, f1, -1.0, 1.0, ALU.mult, ALU.add, [f1k], [f3k])
                P.emit('vector', lambda e, f4=f4, f2=f2: e.tensor_tensor_scan(out=f4, data0=rmask, data1=f2, initial=0.0,
                                                                             op0=ALU.mult, op1=ALU.add), [f2k, 'rmask'], [f4k])
                act(f5, f4, AF.Exp, [f4k], [f5k])
                act(f6, f4, AF.Exp, [f4k], [f6k], scale=-1.0)
                kdv, kdk = kd(h)
                tt('vector', kdv, f3, f6, ALU.mult, [f3k, f6k], [kdk])
                kdlv, kdlk = kdl(h)
                ebl_b = f5.rearrange("p (c s) -> p c s", s=64)[:, :, 63:64].to_broadcast([128, 8, 64])
                tt('vector', kdlv.rearrange("p (c s) -> p c s", s=64), kdv.rearrange("p (c s) -> p c s", s=64), ebl_b,
                   ALU.mult, [kdk, f5k], [kdlk])
                cp('gpsimd', EBL[:, h, :], f5.rearrange("p (c s) -> p c s", s=64)[:, :, 63], [f5k], [('EBL', h)])
                bank, bk = proj_fm(w_aq, k_aq, h * 128, None)
                act(f0, bank, AF.Silu, [bk], [f0k])
                qdv, qdk = qd(h)
                tt('vector', qdv, f0, f5, ALU.mult, [f0k, f5k], [qdk])
        w_ai, k_ai = wnext('ein2')
        if not dry:
            for j in range(4):
                bank, bk = pm_next()
                for kc in range(8):
                    mm(bank, hT[:, kc, j * 128:(j + 1) * 128], w_ai[:, kc, :], kc == 0, kc == 7, [k_ai] + HTK, [bk])
                cp('scalar', Vt[:, j, :], bank, [bk], [('Vt', j)])
        w_ag, k_ag = wnext('ein3')
        if not dry and SUB >= 2:
            for h in range(4):
                bank, bk = proj_fm(w_ag, k_ag, h * 128, None)
                ogv, ogk = og(h)
                act(ogv, bank, AF.Silu, [bk], [ogk])
            for j in range(4):
                p, pk = pt_next()
                for h in range(4):
                    kdlv, kdlk = kdl(h)
                    tr(p[:, h * 128:(h + 1) * 128], kdlv[:, j * 128:(j + 1) * 128], ident, [kdlk, 'ident'], [pk])
                cp('scalar', KTt[:, j, :], p[:, 0:512], [pk], [('KTt', j)])
            obanks = pm4()
            for j in range(4 if SUB >= 3 else 0):
                for h in range(4):
                    qdv, qdk = qd(h)
                    kdv, kdk = kd(h)
                    ob, obk = obanks[h]
                    par = (j * 4 + h) % 2
                    pa, pak = pb[6 + par], ('pb', 6 + par)
                    sl = slice(j * 128, (j + 1) * 128)
                    mm(pa[:, 0:128], kdv[:, sl], qdv[:, sl], True, True, [kdk, qdk], [pak])
                    asb = attn_sb[:, par, :]
                    ask = ('attn', par)
                    tt('vector', asb, pa[:, 0:128], mpair, ALU.mult, [pak, 'mpair'], [ask])
                    vsl = slice(h * 128, (h + 1) * 128)
                    mm(ob[:, sl], Vt[:, j, vsl], asb, True, False, [('Vt', j), ask], [obk])
                    for cc in range(2):
                        ps = slice(cc * 64, (cc + 1) * 64)
                        tsl = slice(j * 128 + cc * 64, j * 128 + (cc + 1) * 64)
                        mm(ob[:, tsl], Sbf[:, h, :], qdv[:, tsl], False, cc == 1, [('Sbf', h), qdk], [obk])
                        mm(pa[:, 128 + cc * 128:256 + cc * 128], KTt[ps, j, vsl], Vt[ps, j, vsl], True, True,
                           [('KTt', j), ('Vt', j)], [pak])
                        stt(Sst[:, h, :], Sst[:, h, :], EBL[:, h, j * 2 + cc:j * 2 + cc + 1], pa[:, 128 + cc * 128:256 + cc * 128],
                            ALU.mult, ALU.add, [('S', h), ('EBL', h), pak], [('S', h)])
                        cp('scalar', Sbf[:, h, :], Sst[:, h, :], [('S', h)], [('Sbf', h)])
            for h in range(4 if SUB >= 4 else 0):
                ob, obk = obanks[h]
                act(osq, ob, AF.Square, [obk], ['osq'])
                f1, f1k = F(1); f2, f2k = F(2)
                ogv, ogk = og(h)
                tt('vector', f1, ob, ogv, ALU.mult, [obk, ogk], [f1k])
                pa, pak = pb[6 + h % 2], ('pb', 6 + h % 2)
                mm(pa, ones, osq, True, True, ['ones', 'osq'], [pak])
                act(f2, pa, AF.Sqrt, [pak], [f2k], scale=1.0 / 128, bias=epsD)
                P.emit('vector', lambda e, f2=f2: e.reciprocal(out=f2, in_=f2), [f2k], [f2k])
                mv, mk = mix(h)
                stt(mv, f1, again[:, h:h + 1], f2, ALU.mult, ALU.mult, [f1k, 'again', f2k], [mk])


    if nlayers > 1:
        kidxT2 = sb("kidxT2", [128, S], BF16)
        ckvT = sb("ckvT", [128, S], BF16)
        ckvtok = sb("ckvtok", [128, S // 128, 128], BF16)
        score = sb("score", [128, S], BF16)
        mk = Fb.rearrange("p a t -> p (a t)").bitcast(BF16)
        FALL = [('F', i) for i in range(8)]
        wuq_sb = sb("wuq_sb", [128, 2, 512], BF16)
        iwq_sb = sb("iwq_sb", [128, 2, 512], BF16)
        wuk_sb = sb("wuk_sb", [128, 4, 128], BF16)
        wuv_sb = sb("wuv_sb", [128, 8, 64], BF16)
        widx_sb = sb("widx_sb", [128, 8, 8], BF16)
        wT_sb = sb("wT_sb", [128, 4, 128], BF16)
        wsl = sb("wsl", [128, 4, 128], BF16)
        bs_bc = sb("bs_bc", [128, 4, 128], BF16)
        vg_bc = sb("vg_bc", [128, 512], BF16)
        vb_bc = sb("vb_bc", [128, 512], BF16)
        cb = sb("cb", [128, 128], F32)
        cqg = sb("cqg", [128, 2], F32)
        ckvg = sb("ckvg", [128, 1], F32)
        kig = sb("kig", [128, 1], F32)
        kib = sb("kib", [128, 1], F32)
        eps5 = sb("eps5", [128, 1], F32)
        wraw = sb("wraw", [128, 4, 8], F32)
        wabs = sb("wabs", [128, 4, 8], F32)
        wsgn = sb("wsgn", [128, 4, 8], F32)
        sm = sb("sm", [128, 16], F32)
        lnst = sb("lnst", [128, 4, 4], F32)
        rden = sb("rden", [128, 8], F32)
        memset('vector', eps5, 1e-5, ['eps5'])
        dma('gpsimd', wuq_sb, Wd['odd_c_w_uq'][0].rearrange("(c p) n -> p c n", p=128), (), ['wuq'])
        dma('gpsimd', iwq_sb, Wd['odd_c_idx_wq'][0].rearrange("(c p) n -> p c n", p=128), (), ['iwq'])
        for hp in range(4):
            dma('gpsimd', wuk_sb[:, hp, :], Wd['odd_c_w_uk'][0][2 * hp:2 * hp + 2].rearrange("h d c -> (h d) c"), (), ['wuk'])
        dma('gpsimd', wuv_sb, Wd['odd_c_w_uv'][0].rearrange("h c d -> c h d"), (), ['wuv'])
        dma('gpsimd', widx_sb, Wd['odd_w_in'][0][:, 448:456].rearrange("(kc p) n -> p kc n", p=128), (), ['widx'], slow=True)
        dma('gpsimd', wsl, Wd['odd_d_w_s'][0].rearrange("g t s -> t g s"), (), ['wsl'])
        for g_ in range(4):
            P.emit('gpsimd', lambda e, g_=g_: e.affine_select(out=wsl[:, g_, :], in_=wsl[:, g_, :], pattern=[[-1, 128]], compare_op=ALU.is_ge,
                                                            fill=0.0, base=0, channel_multiplier=1), ['wsl'], ['wsl'])
        p_, pk_ = pt_next()
        for g_ in range(4):
            tr(p_[:, g_ * 128:(g_ + 1) * 128], wsl[:, g_, :], ident, ['wsl', 'ident'], [pk_])
        cp('vector', wT_sb, p_[:, 0:512].rearrange("p (g t) -> p g t", t=128), [pk_], ['wT'])
        dma('gpsimd', bs_bc.rearrange("p g t -> p (g t)"), Wd['odd_d_b_s'][0].rearrange("g t -> (g t)").partition_broadcast(128), (), ['bsbc'])
        dma('gpsimd', vg_bc, Wd['odd_d_v_g'][0].partition_broadcast(128), (), ['vgbc'])
        dma('gpsimd', vb_bc, Wd['odd_d_v_b'][0].partition_broadcast(128), (), ['vbbc'])
        memset('vector', cb, 0.0, ['cb'])
        P.emit('gpsimd', lambda e: e.affine_select(out=cb, in_=cb, pattern=[[-1, 128]], compare_op=ALU.is_ge,
                                                   fill=NEG, base=0, channel_multiplier=1), ['cb'], ['cb'])
        dma('sync', cqg, Wd['odd_c_q_norm'][0].rearrange("(c p) -> p c", p=128), (), ['cqg'], slow=True)
        dma('sync', ckvg, Wd['odd_c_kv_norm'][0].rearrange("(c p) -> p c", p=128), (), ['ckvg'], slow=True)
        for hh in range(2):
            dma('sync', kig[hh * 64:(hh + 1) * 64, :], Wd['odd_c_idx_k_g'][0].rearrange("(c p) -> p c", p=64), (), ['kig'], slow=True)
            dma('sync', kib[hh * 64:(hh + 1) * 64, :], Wd['odd_c_idx_k_b'][0].rearrange("(c p) -> p c", p=64), (), ['kib'], slow=True)

    NR = 16

    MBDT = mybir.dt.float8e4 if os.environ.get('KMB', 'fp8') == 'fp8' else BF16
    MBV = -224.0
    if nlayers > 1:
        if MBDT == BF16:
            assert S <= 4096
            mbv_ = Fb.rearrange("p a t -> p (a t)").bitcast(BF16)
            mbuf = [mbv_[:, 0:4096], mbv_[:, 4096:8192]]
        else:
            mbv_ = Fb.rearrange("p a t -> p (a t)").bitcast(MBDT)
            mbuf = [mbv_[:, 0:8192], mbv_[:, 8192:16384]]
        MBK = [[('F', i) for i in range(4)], [('F', 4 + i) for i in range(4)]]
        PTb = [hT[:, 0:2, :].rearrange("p a t -> p (a t)"), KTt[:, 0:2, :].rearrange("p a t -> p (a t)")]
        PTKb = [[('hT', 0), ('hT', 1)], [('KTt', 0), ('KTt', 1)]]
        acc_ = hT[:, 2:4, :].rearrange("p a t -> p (a t)").bitcast(F32)
        ACK = [('hT', 2), ('hT', 3)]
        Rh = [hT[:, 4, :], hT[:, 5, :]]
        RHK = [('hT', 4), ('hT', 5)]
        olat = hT[:, 6:8, :].rearrange("p a t -> p (a t)")
        OLK = [('hT', 6), ('hT', 7)]
        qlb = Vt[:, 0:2, :].rearrange("p a t -> p (a t)")
        QLK = [('Vt', 0), ('Vt', 1)]
        otok = Vt[:, 2, :]
        OTK = ('Vt', 2)
        ib_i = [0]
        qk_i = [0]

    def ib_next():
        i = ib_i[0] % 2
        ib_i[0] += 1
        return pb[i], ('pb', i)

    def qk_next():
        i = (2, 4, 5)[qk_i[0] % 3]
        qk_i[0] += 1
        return pb[i], ('pb', i)

    def dsa_indexer(ti, qbl):
        Q = ti * 4 + qbl
        L = (Q + 1) * 128
        qsl = slice(qbl * 128, (qbl + 1) * 128)
        nsb = (L + 511) // 512
        for sbi in range(nsb):
            n = min(512, L - sbi * 512)
            ksl = slice(sbi * 512, sbi * 512 + n)
            kk = [('kidx', sbi)]
            for h in range(8):
                bank, bk = ib_next()
                hp = slice((h % 2) * 64, (h % 2 + 1) * 64)
                mm(bank[:, 0:n], A[hp, 6 + h // 2, qsl], kidxT2[hp, ksl], True, True, [('A', 6 + h // 2)] + kk, [bk])
                r = Rh[h % 2]
                rk = RHK[h % 2]
                act(r[:, 0:n], bank[:, 0:n], AF.Relu, [bk, 'wabs'], [rk], scale=wabs[:, qbl, h:h + 1])
                if h == 0:
                    ts('vector', acc_[:, 0:n], r[:, 0:n], wsgn[:, qbl, 0:1], None, ALU.mult, None, [rk, 'wsgn'], ACK)
                elif h < 7:
                    stt(acc_[:, 0:n], r[:, 0:n], wsgn[:, qbl, h:h + 1], acc_[:, 0:n], ALU.mult, ALU.add, [rk, 'wsgn'] + ACK, ACK)
                else:
                    stt(score[:, ksl], r[:, 0:n], wsgn[:, qbl, h:h + 1], acc_[:, 0:n], ALU.mult, ALU.add, [rk, 'wsgn'] + ACK, ['score'])
        dsl = slice(Q * 128, (Q + 1) * 128)
        tt('vector', score[:, dsl], score[:, dsl], cb, ALU.add, ['score', 'cb'], ['score'])

    def dsa_threshold(ti, qbl):
        Q = ti * 4 + qbl
        L = (Q + 1) * 128
        mb = mbuf[Q % 2]
        mbk = MBK[Q % 2]
        lo = sm[:, 0:1]; hi = sm[:, 1:2]; w0_ = sm[:, 2:3]; mid = sm[:, 3:4]; cnt = sm[:, 4:5]; selw = sm[:, 5:6]
        if Q >= 2:
            P.emit('vector', lambda e: e.tensor_reduce(out=hi, in_=score[:, 0:L], axis=AX.X, op=ALU.max), ['score'], ['hi'])
            P.emit('vector', lambda e: e.tensor_reduce(out=lo, in_=score[:, 0:L - 128], axis=AX.X, op=ALU.min), ['score'], ['lo'])
            tt('vector', w0_, hi, lo, ALU.subtract, ['hi', 'lo'], ['w0'])
            for r_ in range(NR):
                sc_ = 2.0 ** -(r_ + 1)
                stt(mid, w0_, sc_, lo, ALU.mult, ALU.add, ['w0', 'lo'], ['mid'])
                ts('vector', mb[:, 0:L], score[:, 0:L], mid, 0.0, ALU.is_ge, ALU.add, ['score', 'mid'], mbk + ['cnt'], accum=cnt)
                ts('vector', selw, cnt, 256.0, sc_, ALU.is_ge, ALU.mult, ['cnt'], ['selw'])
                stt(lo, selw, w0_, lo, ALU.mult, ALU.add, ['selw', 'w0', 'lo'], ['lo'])
        else:
            memset('vector', lo, NEG * 0.5, ['lo'])
        ts('vector', mb[:, 0:L], score[:, 0:L], lo, MBV, ALU.is_lt, ALU.mult, ['score', 'lo'], mbk)

    def dsa_attention(ti, qbl):
        Q = ti * 4 + qbl
        qsl = slice(qbl * 128, (qbl + 1) * 128)
        mb = mbuf[Q % 2]
        mbk = MBK[Q % 2]
        qb0, qb0k = qk_next()
        qb1, qb1k = qk_next()
        for h in range(8):
            bank, bk = (qb0, qb0k) if h % 2 == 0 else (qb1, qb1k)
            hp = slice((h % 2) * 64, (h % 2 + 1) * 64)
            mm(bank[:, (h // 2) * 128:(h // 2 + 1) * 128], wuk_sb[hp, h // 2, :], A[hp, 2 + h // 2, qsl], True, True,
               ['wuk', ('A', 2 + h // 2)], [bk])
        qlv = qlb.rearrange("p (hp two t) -> p hp two t", two=2, t=128)
        act(qlv[:, :, 0, :], qb0.rearrange("p (h t) -> p h t", t=128), AF.Copy, [qb0k], QLK, scale=0.125)
        act(qlv[:, :, 1, :], qb1.rearrange("p (h t) -> p h t", t=128), AF.Copy, [qb1k], QLK, scale=0.125)
        accA = (pb[6], ('pb', 6)); accB = (pb[7], ('pb', 7)); accC = (pb[3], ('pb', 3))
        for st_ in range(Q + 1):
            ssl = slice(st_ * 128, (st_ + 1) * 128)
            ck = [('ckvT', st_ // 4)]
            PT = PTb[st_ % 2]
            PTK = PTKb[st_ % 2]
            lbs = [qk_next(), qk_next()]
            for half in range(2):
                lb_, lk_ = lbs[half]
                mm(lb_, ckvT[:, ssl], qlb[:, half * 512:(half + 1) * 512], True, False, ck + [QLK[half]], [lk_], sgc=True)
                for hh in range(4):
                    mm(lb_[:, hh * 128:(hh + 1) * 128], mb[:, ssl], ident, False, hh == 3, mbk + ['ident'], [lk_], sgc=True)
                act(PT[:, half * 512:(half + 1) * 512], lb_, AF.Exp, [lk_], [PTK[half]])
            ctk = [('ckvtok', st_ // 4)]
            first = (st_ == 0)
            last = (st_ == Q)
            mm(accA[0][:, 0:384], ckvtok[:, st_, :], PT[:, 0:384], first, last, ctk + PTK, [accA[1]])
            mm(accB[0][:, 0:384], ckvtok[:, st_, :], PT[:, 384:768], first, last, ctk + PTK, [accB[1]])
            mm(accC[0][:, 0:256], ckvtok[:, st_, :], PT[:, 768:1024], first, last, ctk + PTK, [accC[1]], sgc=True)
            for h in range(8):
                mm(accC[0][:, 256 + h:257 + h], PT[:, h * 128:(h + 1) * 128], ones[:, 0:1], False, last, PTK + ['ones'], [accC[1]], sgc=True)
        act(olat[:, 0:384], accA[0][:, 0:384], AF.Copy, [accA[1]], OLK)
        act(olat[:, 384:768], accB[0][:, 0:384], AF.Copy, [accB[1]], OLK)
        act(olat[:, 768:1024], accC[0][:, 0:256], AF.Copy, [accC[1]], OLK)
        P.emit('vector', lambda e, src_=accC[0][:, 256:264]: e.reciprocal(out=rden, in_=src_), [accC[1]], ['rden'])
        ob_, obk_ = qk_next()
        for h in range(8):
            mm(ob_[:, h * 64:(h + 1) * 64], olat[:, h * 128:(h + 1) * 128], wuv_sb[:, h, :], True, True, OLK + ['wuv'], [obk_])
        tt('vector', otok.rearrange("p (h d) -> p h d", d=64), ob_.rearrange("p (h d) -> p h d", d=64),
           rden.unsqueeze(2).to_broadcast([128, 8, 64]), ALU.mult, [obk_, 'rden'], [OTK])
        p, pk = qk_next()
        p = p.bitcast(BF16)
        for k_ in range(4):
            tr(p[:, k_ * 128:(k_ + 1) * 128], otok[:, k_ * 128:(k_ + 1) * 128], ident, [OTK, 'ident'], [pk])
        MK4 = [('A', 10 + c) for c in range(4)]
        cp('scalar', A[:, 10:14, qsl], p[:, 0:512].rearrange("p (k t) -> p k t", t=128), [pk], MK4)


    def layer1(ti):
        dry = wstate['dry']
        if not dry:
            norm_T(xs, 4, G_OMIX, hT, HTK, XK)
        w0, k0 = wnext('oin0')
        tsl_ = slice(ti * T, (ti + 1) * T)
        if not dry:
            sqb = [PTx[:, 0, :], PTx[:, 1, :], osq, A[:, 10, :]]
            sqk = [('PTx', 0), ('PTx', 1), 'osq', ('A', 10)]
            raw = [F(0), F(1), F(3), F(5)]
            cols = [0, 128, 256, 384]
            for i in range(4):
                bank, bk = proj_fm(w0, k0, cols[i], None)
                act(sqb[i], bank, AF.Square if i < 3 else AF.Copy, [bk], [sqk[i]])
                cp('vector', raw[i][0], bank, [bk], [raw[i][1]])
            pq, pqk = pb[6], ('pb', 6)
            pc, pck = pb[7], ('pb', 7)
            pi, pik = pb[3], ('pb', 3)
            for c in range(2):
                mm(pq, ones, sqb[c], c == 0, c == 1, ['ones', sqk[c]], [pqk])
            mm(pc, ones, sqb[2], True, True, ['ones', sqk[2]], [pck])
            mm(pi, ones, sqb[3], True, True, ['ones', sqk[3]], [pik])
            f2, f2k = F(2); f3, f3k = F(3); f4, f4k = F(4); f5, f5k = F(5); f6, f6k = F(6)
            act(f2, pq, AF.Sqrt, [pqk], [f2k], scale=1.0 / 256, bias=epsD)
            P.emit('vector', lambda e, f2=f2: e.reciprocal(out=f2, in_=f2), [f2k], [f2k])
            for c in range(2):
                f, fk = F(c)
                stt(A[:, c, :], f, cqg[:, c:c + 1], f2, ALU.mult, ALU.mult, [fk, 'cqg', f2k], [('A', c)])
            act(f4, pc, AF.Sqrt, [pck], [f4k], scale=1.0 / 128, bias=epsD)
            P.emit('vector', lambda e, f4=f4: e.reciprocal(out=f4, in_=f4), [f4k], [f4k])
            stt(ckvT[:, tsl_], f3, ckvg[:, 0:1], f4, ALU.mult, ALU.mult, [f3k, 'ckvg', f4k], [('ckvT', ti)])
            stt(f5, pi, -1.0 / 128, f5, ALU.mult, ALU.add, [pik, f5k], [f5k])
            act(A[:, 11, :], f5, AF.Square, [f5k], [('A', 11)])
            for j in range(4):
                bank, bk = pm_next()
                for kc in range(8):
                    mm(bank[:, 0:8], hT[:, kc, j * 128:(j + 1) * 128], widx_sb[:, kc, :], kc == 0, kc == 7, ['widx'] + HTK, [bk])
                cp('vector', wraw[:, j, :], bank[:, 0:8], [bk], [('wraw', j)])
            WRK = [('wraw', j) for j in range(4)]
            act(wabs, wraw, AF.Abs, WRK, ['wabs'], scale=(8.0 ** -0.5) * 0.125)
            act(wsgn, wraw, AF.Sign, WRK, ['wsgn'])
        w1, k1 = wnext('oin1')
        if not dry:
            for c in range(4):
                bank, bk = proj_fm(w1, k1, c * 128, None)
                act(A[:, 14 + c, :], bank, AF.Gelu, [bk], [('A', 14 + c)])
            mm(pi, ones, A[:, 11, :], True, True, ['ones', ('A', 11)], [pik])
            act(f6, pi, AF.Sqrt, [pik], [f6k], scale=1.0 / 128, bias=eps5)
            P.emit('vector', lambda e, f6=f6: e.reciprocal(out=f6, in_=f6), [f6k], [f6k])
            tt('vector', f5, f5, f6, ALU.mult, [f5k, f6k], [f5k])
            ts('vector', kidxT2[:, tsl_], f5, kig[:, 0:1], kib[:, 0:1], ALU.mult, ALU.add, [f5k, 'kig', 'kib'], [('kidx', ti)])
            p, pk = pt_next()
            for j in range(4):
                tr(p[:, j * 128:(j + 1) * 128], ckvT[:, ti * T + j * 128: ti * T + (j + 1) * 128], ident, [('ckvT', ti), 'ident'], [pk])
            cp('scalar', ckvtok[:, ti * 4:(ti + 1) * 4, :], p[:, 0:512].rearrange("p (j c) -> p j c", c=128), [pk], [('ckvtok', ti)])
            for oc in range(4):
                bank, bk = pm_next()
                for c in range(2):
                    mm(bank, wuq_sb[:, c, oc * 128:(oc + 1) * 128], A[:, c, :], c == 0, c == 1, ['wuq', ('A', c)], [bk])
                cp('scalar', A[:, 2 + oc, :], bank, [bk], [('A', 2 + oc)])
                bank, bk = pm_next()
                for c in range(2):
                    mm(bank, iwq_sb[:, c, oc * 128:(oc + 1) * 128], A[:, c, :], c == 0, c == 1, ['iwq', ('A', c)], [bk])
                cp('scalar', A[:, 6 + oc, :], bank, [bk], [('A', 6 + oc)])
        mark('l1_prep_done')
        w2_, k2 = wnext('oin2')
        if not dry and KL1 >= 2:
            for j in range(4):
                bank, bk = pm_next()
                for kc in range(8):
                    mm(bank, hT[:, kc, j * 128:(j + 1) * 128], w2_[:, kc, :], kc == 0, kc == 7, [k2] + HTK, [bk])
                f, fk = F(j % 2)
                g, gk = F(2 + j % 2)
                act(f, bank, AF.Gelu, [bk], [fk, ('lnst', j)], accum=lnst[:, j, 0:1])
                ts('vector', lnst[:, j, 1:2], lnst[:, j, 0:1], -1.0 / 512, None, ALU.mult, None, [('lnst', j)], [('lnst', j)])
                ts('vector', f, f, lnst[:, j, 1:2], None, ALU.add, None, [fk, ('lnst', j)], [fk])
                act(g, f, AF.Square, [fk], [gk, ('lnst', j)], accum=lnst[:, j, 2:3])
                act(lnst[:, j, 3:4], lnst[:, j, 2:3], AF.Sqrt, [('lnst', j)], [('lnst', j)], scale=1.0 / 512, bias=eps5)
                P.emit('vector', lambda e, j=j: e.reciprocal(out=lnst[:, j, 3:4], in_=lnst[:, j, 3:4]), [('lnst', j)], [('lnst', j)])
                stt(g, f, lnst[:, j, 3:4], vg_bc, ALU.mult, ALU.mult, [fk, ('lnst', j), 'vgbc'], [gk])
                tt('vector', KTt[:, j, :], g, vb_bc, ALU.add, [gk, 'vbbc'], [('KTt', j)])
                bank, bk = pm_next()
                for g_ in range(4):
                    mm(bank[:, g_ * 128:(g_ + 1) * 128], KTt[:, j, g_ * 128:(g_ + 1) * 128], wT_sb[:, g_, :], True, True, [('KTt', j), 'wT'], [bk])
                tt('vector', f, bank, bs_bc.rearrange("p g t -> p (g t)"), ALU.add, [bk, 'bsbc'], [fk])
                av = A[:, 14:18, j * 128:(j + 1) * 128]
                AK = [('A', 14 + c) for c in range(4)]
                tt('vector', av, f.rearrange("p (g t) -> p g t", t=128), av, ALU.mult, [fk] + AK, AK)
            mark('l1_gmlp_done')
            order = ['i0', 't0', 'i1', 't1', 'a0', 'i2', 't2', 'a1', 'i3', 't3', 'a2', 'a3']
            if os.environ.get('KSEQ', '0') == '1':
                order = ['i0', 't0', 'a0', 'i1', 't1', 'a1', 'i2', 't2', 'a2', 'i3', 't3', 'a3']
            for step in order:
                qbl = int(step[1])
                if KL1 < 3 or (KL1 == 3 and step[0] != 'i') or (KL1 == 4 and step[0] == 'a'):
                    continue
                if step[0] == 'i':
                    dsa_indexer(ti, qbl)
                elif step[0] == 't':
                    dsa_threshold(ti, qbl)
                else:
                    dsa_attention(ti, qbl)
        mark('l1_dsa_done')
        if os.environ.get('KSTOP', '0') == '1':
            return
        resid_add(lambda nb, kg: f'oout{nb}', A[:, 10:18, :], [('A', 10 + c) for c in range(8)], [8])

    gfin = Fb[:, 0:2, :].rearrange("p a t -> p (a t)")

    wstate['dry'] = True
    wstate['pos'] = 0
    for ti in range(NT):
        tile_body(ti)
    wstate['dry'] = False
    wstate['pos'] = 0
    dmy = sb("dmy", [128, 4], F32)
    P.barrier({
        'vector': (lambda e: e.memset(dmy[:, 0:1], 0.0), ['dmy0']),
        'gpsimd': (lambda e: e.memset(dmy[:, 1:2], 0.0), ['dmy1']),
        'scalar': (lambda e: e.activation(out=dmy[:, 2:3], in_=epsD, func=AF.Copy), ['dmy2']),
        'tensor': (lambda e: e.matmul(pb[0][0:1, 0:1], lhsT=ones[:, 0:1], rhs=ones[:, 0:1], start=True, stop=True), [('pb', 0)]),
    })
    emit_casts([f'wkv{l}_{nb}' for l in range(nlayers) for nb in range(4)])
    emit_casts(list(wseq))
    pending.clear()
    for l in range(nlayers):
        mem_kv(l)
    for ti in range(NT):
        tile_body(ti)
    P.finalize()
    return nc


_CACHE = {}
MARKS = []


def kernel(**inputs):
    x = np.ascontiguousarray(inputs['x'], dtype=np.float32)
    mem = np.ascontiguousarray(inputs['mem'], dtype=np.float32)
    B, S, _ = x.shape
    if S not in _CACHE:
        _CACHE[S] = build(S)
    nc = _CACHE[S]
    in_maps = []
    for b in range(B):
        m = {"x": x[b], "mem": mem[b]}
        for name, shp in WEIGHT_SPECS:
            m[name] = np.ascontiguousarray(inputs[name], dtype=np.float32)
        in_maps.append(m)
    res = run_bass_kernel_spmd(nc, in_maps, core_ids=list(range(B)))
    return np.stack([r["y"] for r in res.results], axis=0)
```

```python
import contextlib
import numpy as np
import concourse.bass as bass
import concourse.mybir as mybir
from concourse.bass_utils import run_bass_kernel_spmd

F32 = mybir.dt.float32
BF16 = mybir.dt.bfloat16
AF = mybir.ActivationFunctionType
ALU = mybir.AluOpType
AX = mybir.AxisListType

EPOCH = 30000
NSLOT = 16
D = 1024
T = 512
FF = 2816
NEG = -1.0e30
import os
STAGE = int(os.environ.get('KSTAGE', '9'))
SUB = int(os.environ.get('KSUB', '9'))
KL1 = int(os.environ.get('KL1', '9'))


class Prog:
    ENGS = ['tensor', 'vector', 'scalar', 'gpsimd', 'sync']

    def __init__(self, nc):
        self.nc = nc
        self.q = {e: [] for e in self.ENGS}
        self.cnt = {e: 0 for e in self.ENGS}
        self.dcnt = {e: 0 for e in self.ENGS}
        self.known = {e: {} for e in self.ENGS}
        self.last_w = {}
        self.readers = {}

    def _need(self, waits, ev, eng, raw):
        if ev is None:
            return
        kind, s, n = ev
        if kind == 'E' and s == eng:
            if eng == 'tensor':
                return
        k = (kind, s)
        if self.known[eng].get(k, 0) >= n:
            return
        if waits.get(k, 0) < n:
            waits[k] = n

    def emit(self, eng, fn, reads=(), writes=(), dma=False, extra=()):
        pr = [k for k in reads if isinstance(k, tuple) and k[0] == 'pb']
        if pr:
            reads = [k for k in reads if not (isinstance(k, tuple) and k[0] == 'pb')]
            writes = list(writes) + [k for k in pr if k not in writes]
        waits = {}
        for ev in extra:
            self._need(waits, ev, eng, True)
        for k in reads:
            self._need(waits, self.last_w.get(k), eng, True)
        for k in writes:
            self._need(waits, self.last_w.get(k), eng, False)
            for kk, n in self.readers.get(k, {}).items():
                self._need(waits, (kk[0], kk[1], n), eng, False)
        if dma:
            i = self.dcnt[eng]
            self.dcnt[eng] = i + 1
            slot = i % NSLOT
            n = i // NSLOT + 1
            if n > 1:
                self._need(waits, ('D', (eng, slot), n - 1), eng, False)
            ev = ('D', (eng, slot), n)
        else:
            n = self.cnt[eng] + 1
            self.cnt[eng] = n
            ev = ('E', eng, n)
        for k, n_ in waits.items():
            self.known[eng][k] = n_
        kk = (ev[0], ev[1])
        for k in reads:
            d = self.readers.setdefault(k, {})
            if d.get(kk, 0) < ev[2]:
                d[kk] = ev[2]
        for k in writes:
            self.last_w[k] = ev
            self.readers[k] = {}
        self.q[eng].append((list(waits.items()), fn, ev))
        return ev

    def all_events(self):
        evs = []
        for e in self.ENGS:
            if self.cnt[e] > 0:
                evs.append(('E', e, self.cnt[e]))
            for slot in range(min(NSLOT, self.dcnt[e])):
                evs.append(('D', (e, slot), (self.dcnt[e] - 1 - slot) // NSLOT + 1))
        return evs

    def barrier(self, dummies):
        evs = self.all_events()
        for eng, (fn, writes) in dummies.items():
            self.emit(eng, fn, (), writes, extra=evs)

    def finalize(self):
        nc = self.nc
        st = contextlib.ExitStack()
        esem = {}
        for e in self.ENGS:
            ne = self.cnt[e] // EPOCH + 1
            esem[e] = [st.enter_context(nc.semaphore(f"s_{e}_{j}")) for j in range(ne)]
        dsem = {}
        per = EPOCH // 16
        for e in self.ENGS:
            for slot in range(min(NSLOT, self.dcnt[e])):
                nd = (self.dcnt[e] + NSLOT - 1) // NSLOT
                ne = nd // per + 1
                dsem[(e, slot)] = [st.enter_context(nc.semaphore(f"d_{e}_{slot}_{j}")) for j in range(ne)]

        def hw(kind, s, n):
            if kind == 'E':
                return esem[s][(n - 1) // EPOCH], (n - 1) % EPOCH + 1
            return dsem[s][(n - 1) // per], ((n - 1) % per + 1) * 16

        with nc.Block() as block:
            def run(engname, engobj):
                for waits, fn, ev in self.q[engname]:
                    for (kind, s), n in waits:
                        sem, val = hw(kind, s, n)
                        engobj.wait_ge(sem, val)
                    inst = fn(engobj)
                    sem, val = hw(*ev)
                    inst.then_inc(sem, 16 if ev[0] == 'D' else 1)
                if engname == 'sync':
                    for e in self.ENGS:
                        if self.cnt[e] > 0:
                            sem, val = hw('E', e, self.cnt[e])
                            engobj.wait_ge(sem, val)
                        for slot in range(min(NSLOT, self.dcnt[e])):
                            nlast = (self.dcnt[e] - 1 - slot) // NSLOT + 1
                            sem, val = hw('D', (e, slot), nlast)
                            engobj.wait_ge(sem, val)

            @block.tensor
            def _(eng):
                run('tensor', eng)

            @block.vector
            def _(eng):
                run('vector', eng)

            @block.scalar
            def _(eng):
                run('scalar', eng)

            @block.gpsimd
            def _(eng):
                run('gpsimd', eng)

            @block.sync
            def _(eng):
                run('sync', eng)
        st.close()


WEIGHT_SPECS = [
    ('hgrn_lower_bounds', [3, 512]), ('even_mix_norm', [1, 1024]), ('even_w_in', [1, 1024, 3584]),
    ('even_a_out_norm', [1, 4, 128]), ('even_b_conv', [1, 3, 512]), ('even_w_out', [1, 1024, 1024]),
    ('odd_mix_norm', [1, 1024]), ('odd_w_in', [1, 1024, 1480]), ('odd_c_q_norm', [1, 256]),
    ('odd_c_kv_norm', [1, 128]), ('odd_c_w_uq', [1, 256, 512]), ('odd_c_w_uk', [1, 8, 64, 128]),
    ('odd_c_w_uv', [1, 8, 128, 64]), ('odd_c_idx_wq', [1, 256, 512]), ('odd_c_idx_k_g', [1, 64]),
    ('odd_c_idx_k_b', [1, 64]), ('odd_d_v_g', [1, 512]), ('odd_d_v_b', [1, 512]),
    ('odd_d_w_s', [1, 4, 128, 128]), ('odd_d_b_s', [1, 4, 128]), ('odd_w_out', [1, 1024, 1024]),
    ('xa_norm', [2, 1024]), ('xa_mem_norm', [2, 1024]), ('xa_wq', [2, 1024, 1024]),
    ('xa_wkv', [2, 1024, 2048]), ('xa_wo', [2, 1024, 1024]), ('ffn_norm', [2, 1024]),
    ('ffn_w13', [2, 1024, 5632]), ('ffn_w2', [2, 2816, 1024]), ('final_norm', [1024]),
]


def build(S, nlayers=2):
    NT = S // T
    nc = bass.Bass("TRN2", target_bir_lowering=False)
    P = Prog(nc)
    x_d = nc.dram_tensor("x", [S, D], F32, kind="ExternalInput").ap()
    mem_d = nc.dram_tensor("mem", [256, D], F32, kind="ExternalInput").ap()
    y_d = nc.dram_tensor("y", [S, D], F32, kind="ExternalOutput").ap()
    Wd = {}
    for name, shp in WEIGHT_SPECS:
        Wd[name] = nc.dram_tensor(name, shp, F32, kind="ExternalInput").ap()

    def sb(name, shape, dt):
        return nc.alloc_sbuf_tensor(name, shape, dt).ap()

    def mm(out, lhsT, rhs, start, stop, reads, writes, sgc=False):
        P.emit('tensor', lambda e: e.matmul(out, lhsT=lhsT, rhs=rhs, start=start, stop=stop, skip_group_check=sgc), reads, writes)

    def tr(out, in_, ident, reads, writes):
        P.emit('tensor', lambda e: e.transpose(out=out, in_=in_, identity=ident), reads, writes)

    def act(out, in_, func, reads, writes, scale=None, bias=None, accum=None):
        kw = {}
        if scale is not None:
            kw['scale'] = scale
        if bias is not None:
            kw['bias'] = bias
        if accum is not None:
            kw['accum_out'] = accum
        P.emit('scalar', lambda e: e.activation(out=out, in_=in_, func=func, **kw), reads, writes)

    def ts(eng, out, in0, s1, s2, op0, op1, reads, writes, accum=None):
        kw = {}
        if accum is not None:
            kw['accum_out'] = accum
        if out.dtype == mybir.dt.float8e4:
            kw['saturate'] = False
        if op1 is None:
            P.emit(eng, lambda e: e.tensor_scalar(out=out, in0=in0, scalar1=s1, scalar2=None, op0=op0, **kw), reads, writes)
        else:
            P.emit(eng, lambda e: e.tensor_scalar(out=out, in0=in0, scalar1=s1, scalar2=s2, op0=op0, op1=op1, **kw), reads, writes)

    def tt(eng, out, in0, in1, op, reads, writes):
        P.emit(eng, lambda e: e.tensor_tensor(out=out, in0=in0, in1=in1, op=op), reads, writes)

    def stt(out, in0, scalar, in1, op0, op1, reads, writes):
        P.emit('vector', lambda e: e.scalar_tensor_tensor(out=out, in0=in0, scalar=scalar, in1=in1, op0=op0, op1=op1), reads, writes)

    def cp(eng, out, in_, reads, writes):
        if eng == 'scalar':
            P.emit(eng, lambda e: e.copy(out=out, in_=in_), reads, writes)
        else:
            P.emit(eng, lambda e: e.tensor_copy(out=out, in_=in_), reads, writes)

    def memset(eng, ap, val, writes):
        P.emit(eng, lambda e: e.memset(ap, val), (), writes)

    def dma(eng, out, in_, reads, writes, slow=False):
        if slow:
            P.emit(eng, lambda e: e.dma_start(out=out, in_=in_, allow_slow_non_contiguous=True), reads, writes, dma=True)
        else:
            P.emit(eng, lambda e: e.dma_start(out=out, in_=in_), reads, writes, dma=True)

    wblocks = {}
    cast_i = [0]

    def make_block(key, pieces, kcn, ncols):
        wb = nc.dram_tensor("wb_" + key, [128, kcn, ncols], BF16).ap()
        pending[key] = (wb, pieces)
        wblocks[key] = (wb, kcn, ncols)

    pending = {}

    def emit_casts(order):
        for key in order:
            if key not in pending:
                continue
            wb, pieces = pending.pop(key)
            for src, c0 in pieces:
                n = src.shape[1]
                dma('gpsimd', wb[:, :, c0:c0 + n], src.rearrange("(kc p) n -> p kc n", p=128), (), [('wb', key)])

    for nb in range(7):
        make_block(f'ein{nb}', [(Wd['even_w_in'][0][:, nb * 512:(nb + 1) * 512], 0)], 8, 512)
    for nb in range(2):
        make_block(f'eout{nb}', [(Wd['even_w_out'][0][:, nb * 512:(nb + 1) * 512], 0)], 8, 512)
    for l in range(2):
        for nb in range(2):
            make_block(f'wq{l}_{nb}', [(Wd['xa_wq'][l][:, nb * 512:(nb + 1) * 512], 0)], 8, 512)
            make_block(f'wo{l}_{nb}', [(Wd['xa_wo'][l][:, nb * 512:(nb + 1) * 512], 0)], 8, 512)
        for nb in range(4):
            make_block(f'wkv{l}_{nb}', [(Wd['xa_wkv'][l][:, nb * 512:(nb + 1) * 512], 0)], 8, 512)
        for p_ in range(11):
            make_block(f'w13_{l}_{p_}', [(Wd['ffn_w13'][l][:, p_ * 256:(p_ + 1) * 256], 0),
                                         (Wd['ffn_w13'][l][:, FF + p_ * 256:FF + (p_ + 1) * 256], 256)], 8, 512)
        for half, (c0, groups) in enumerate([(0, [8, 2]), (10, [8, 4])]):
            for nb in range(2):
                k0 = c0
                for kg, kn in enumerate(groups):
                    make_block(f'w2_{l}_{half}_{nb}_{kg}', [(Wd['ffn_w2'][l][k0 * 128:(k0 + kn) * 128, nb * 512:(nb + 1) * 512], 0)], kn, 512)
                    k0 += kn
    if nlayers > 1:
        make_block('oin0', [(Wd['odd_w_in'][0][:, 0:448], 0), (Wd['odd_w_in'][0][:, 384:448], 448)], 8, 512)
        make_block('oin1', [(Wd['odd_w_in'][0][:, 456:968], 0)], 8, 512)
        make_block('oin2', [(Wd['odd_w_in'][0][:, 968:1480], 0)], 8, 512)
        for nb in range(2):
            make_block(f'oout{nb}', [(Wd['odd_w_out'][0][:, nb * 512:(nb + 1) * 512], 0)], 8, 512)

    ident = sb("ident", [128, 128], BF16)
    ones = sb("ones", [128, 128], BF16)
    xs = sb("xs", [128, 4, D], F32)
    xn = sb("xn", [128, 1, D], BF16)
    hT = sb("hT", [128, 8, T], BF16)
    A = sb("A", [128, 18, T], BF16)
    Fb = sb("F", [128, 8, T], F32)
    Vt = sb("Vt", [128, 4, T], BF16)
    KTt = sb("KTt", [128, 4, T], BF16)
    WR = sb("WR", [128, 4, 8, 512], BF16)
    gall = sb("gall", [128, 7, 8], F32)
    ss = sb("ss", [128, 4], F32)
    rs = sb("rs", [128, 4], F32)
    xaK = sb("xaK", [128, 2, 8, 256], BF16)
    xaV = sb("xaV", [128, 2, 2, D], BF16)
    Sst = sb("Sst", [128, 4, 128], F32)
    Sbf = sb("Sbf", [128, 4, 128], BF16)
    lbr = sb("lbr", [128, 4, 3], F32)
    lbs = sb("lbs", [128, 4], F32)
    lb = sb("lb", [128, 4], F32)
    oml = sb("oml", [128, 4], F32)
    again = sb("again", [128, 4], F32)
    convw = sb("convw", [128, 4, 3], F32)
    zc = sb("zc", [128, 4, T + 2], BF16)
    EBL = sb("EBL", [128, 4, 8], F32)
    mpair = sb("mpair", [128, 128], F32)
    rmask = sb("rmask", [128, T], BF16)
    attn4 = sb("attn4", [128, 4, 128], BF16)
    osq = sb("osq", [128, T], BF16)
    PTx = sb("PTx", [128, 2, T], BF16)
    junk = PTx.rearrange("p a t -> p (a t)")
    JK = [('PTx', 0), ('PTx', 1)]

    pb = [nc.alloc_psum_tensor(f"pb{i}", [128, 512], F32).ap() for i in range(8)]
    pm_i = [0]

    PMB = (0, 1, 2)

    def pm_next():
        i = PMB[pm_i[0] % len(PMB)]
        pm_i[0] += 1
        return pb[i], ('pb', i)

    def pm4():
        return [(pb[i], ('pb', i)) for i in range(4)]

    pt_i = [0]

    def pt_next():
        i = 4 + pt_i[0] % 2
        pt_i[0] += 1
        return pb[i].bitcast(BF16), ('pb', i)

    memset('gpsimd', ident, 0.0, ['ident'])
    P.emit('gpsimd', lambda e: e.affine_select(out=ident, in_=ident, pattern=[[-1, 128]], compare_op=ALU.not_equal,
                                               fill=1.0, base=0, channel_multiplier=1), ['ident'], ['ident'])
    memset('vector', ones, 1.0, ['ones'])
    memset('vector', mpair, 1.0, ['mpair'])
    P.emit('gpsimd', lambda e: e.affine_select(out=mpair, in_=mpair, pattern=[[1, 128]], compare_op=ALU.is_ge,
                                               fill=0.0, base=0, channel_multiplier=-1), ['mpair'], ['mpair'])
    memset('gpsimd', mpair[0:64, 64:128], 0.0, ['mpair'])
    memset('vector', rmask, 1.0, ['rmask'])
    memset('vector', rmask.rearrange("p (c s) -> p c s", s=64)[:, :, 0:1], 0.0, ['rmask'])
    memset('vector', zc, 0.0, [('zc', c) for c in range(4)])
    memset('vector', Sst, 0.0, [('S', h) for h in range(4)])
    memset('vector', Sbf, 0.0, [('Sbf', h) for h in range(4)])

    gsrc = [Wd['even_mix_norm'][0], Wd['xa_norm'][0], Wd['ffn_norm'][0], Wd['odd_mix_norm'][0],
            Wd['xa_norm'][1], Wd['ffn_norm'][1], Wd['final_norm']]
    for i, g in enumerate(gsrc):
        dma('sync', gall[:, i, :], g.rearrange("(kc p) -> p kc", p=128), (), ['gall'], slow=True)
    G_EMIX, G_XA0, G_FFN0, G_OMIX, G_XA1, G_FFN1, G_FIN = range(7)
    for h in range(4):
        dma('sync', lbr[:, h, :], Wd['hgrn_lower_bounds'][:, h * 128:(h + 1) * 128].rearrange("s k -> k s"), (), ['lbr'], slow=True)
    dma('sync', again, Wd['even_a_out_norm'][0].rearrange("h v -> v h"), (), ['again'], slow=True)
    for c in range(4):
        dma('sync', convw[:, c, :], Wd['even_b_conv'][0][:, c * 128:(c + 1) * 128].rearrange("k p -> p k"), (), ['convw'], slow=True)
    act(lbr, lbr, AF.Exp, ['lbr'], ['lbr'])
    P.emit('vector', lambda e: e.reduce_sum(out=lbs, in_=lbr, axis=AX.X), ['lbr'], ['lbs'])
    P.emit('vector', lambda e: e.reciprocal(out=lbs, in_=lbs), ['lbs'], ['lbs'])
    tt('vector', lb, lbr[:, :, 0], lbs, ALU.mult, ['lbr', 'lbs'], ['lb'])
    ts('vector', oml, lb, -1.0, 1.0, ALU.mult, ALU.add, ['lb'], ['oml'])

    wseq = []
    wstate = {'pos': 0, 'loaded': 0, 'dry': True}

    def wload(m):
        key = wseq[m]
        wb, kcn, ncols = wblocks[key]
        slot = m % 4
        dma('sync', WR[:, slot, 0:kcn, 0:ncols], wb, [('wb', key)], [('W', slot)])

    def wnext(key, keep_prev=False):
        i = wstate['pos']
        wstate['pos'] = i + 1
        if wstate['dry']:
            wseq.append(key)
            return None, None
        assert wseq[i] == key, (i, wseq[i], key)
        lim = min(len(wseq) - 1, i + (1 if keep_prev else 2))
        while wstate['loaded'] <= lim:
            wload(wstate['loaded'])
            wstate['loaded'] += 1
        slot = i % 4
        return WR[:, slot], ('W', slot)

    def norm_T(src, nsub, gidx, dst, dkeys, skeys):
        for j in range(nsub):
            act(junk, src[:, j, :], AF.Square, [skeys[j]], JK + [('ss', j)], accum=ss[:, j:j + 1])
        act(rs[:, 0:nsub], ss[:, 0:nsub], AF.Sqrt, [('ss', j) for j in range(nsub)], ['rs'], scale=1.0 / D, bias=epsD)
        P.emit('vector', lambda e: e.reciprocal(out=rs[:, 0:nsub], in_=rs[:, 0:nsub]), ['rs'], ['rs'])
        for j in range(nsub):
            act(xn[:, 0, :], src[:, j, :], AF.Copy, [skeys[j], 'rs'], [('xn', 0)], scale=rs[:, j:j + 1])
            p, pk = pt_next()
            for kc in range(8):
                tr(p[:, kc * 128:(kc + 1) * 128], xn[:, 0, kc * 128:(kc + 1) * 128], ident, [('xn', 0), 'ident'], [pk])
            tt('vector', dst[:, :, j * 128:(j + 1) * 128], p.rearrange("p (k t) -> p k t", t=128),
               gall[:, gidx, :].unsqueeze(2).to_broadcast([128, 8, 128]), ALU.mult, [pk, 'gall'], dkeys)

    HTK = [('hT', kc) for kc in range(8)]
    XK = [('x', j) for j in range(4)]

    def proj_fm(wv, wk, col0, handler_bank):
        bank, bk = pm_next()
        for kc in range(8):
            mm(bank, wv[:, kc, col0:col0 + 128], hT[:, kc, :], kc == 0, kc == 7, [wk] + HTK, [bk])
        return bank, bk

    def resid_add(wkeys_fn, inT, inkeys, nkc_groups):
        for nb in range(2):
            banks = pm4()
            kc0 = 0
            ng = len(nkc_groups)
            for kg in range(ng):
                wv, wk = wnext(wkeys_fn(nb, kg))
                if wstate['dry']:
                    continue
                kn = nkc_groups[kg]
                for j in range(4):
                    bank, bk = banks[j]
                    for kc in range(kn):
                        mm(bank, inT[:, kc0 + kc, j * 128:(j + 1) * 128], wv[:, kc, :], kc0 + kc == 0,
                           (kg == ng - 1 and kc == kn - 1), [wk] + inkeys, [bk])
                kc0 += kn
            if wstate['dry']:
                continue
            for j in range(4):
                bank, bk = banks[j]
                tt('vector', xs[:, j, nb * 512:(nb + 1) * 512], xs[:, j, nb * 512:(nb + 1) * 512], bank, ALU.add,
                   [bk, ('x', j)], [('x', j)])

    def F(i):
        return Fb[:, i, :], ('F', i)

    epsD = sb("epsD", [128, 1], F32)
    memset('vector', epsD, 1e-6, ['eps'])

    def conv_branch(mix):
        dry = wstate['dry']
        zh = lambda c: (A[:, 8 + c, :], ('A', 8 + c))
        w_bh, k_bh = wnext('ein6')
        if not dry:
            for c in range(4):
                bank, bk = proj_fm(w_bh, k_bh, c * 128, None)
                zv, zk = zh(c)
                cp('scalar', zv, bank, [bk], [zk])
        w_bc, k_bc = wnext('ein5')
        if not dry:
            for c in range(4):
                bank, bk = proj_fm(w_bc, k_bc, c * 128, None)
                zv, zk = zh(c)
                tt('vector', zc[:, c, 2:T + 2], bank, zv, ALU.mult, [bk, zk], [('zc', c)])
        w_bb, k_bb = wnext('ein4')
        if not dry:
            for c in range(4):
                bank, bk = proj_fm(w_bb, k_bb, c * 128, None)
                f3, f3k = F(3); f4, f4k = F(4)
                ts('vector', f3, zc[:, c, 0:T], convw[:, c, 0:1], None, ALU.mult, None, [('zc', c), 'convw'], [f3k])
                stt(f4, zc[:, c, 1:T + 1], convw[:, c, 1:2], f3, ALU.mult, ALU.add, [('zc', c), 'convw', f3k], [f4k])
                stt(f3, zc[:, c, 2:T + 2], convw[:, c, 2:3], f4, ALU.mult, ALU.add, [('zc', c), 'convw', f4k], [f3k])
                mv, mk = mix(4 + c)
                tt('vector', mv, bank, f3, ALU.mult, [bk, f3k], [mk])
                cp('gpsimd', zc[:, c, 0:2], zc[:, c, T:T + 2], [('zc', c)], [('zc', c)])

    def xattn(l, gidx):
        dry = wstate['dry']
        if not dry:
            norm_T(xs, 4, gidx, hT, HTK, XK)
        qT = lambda c: (A[:, c, :], ('A', c))
        oT = lambda c: (A[:, 8 + c, :], ('A', 8 + c))
        for nb in range(2):
            wv, wk = wnext(f'wq{l}_{nb}')
            if dry:
                continue
            for c in range(4):
                bank, bk = proj_fm(wv, wk, c * 128, None)
                qv, qk = qT(nb * 4 + c)
                cp('scalar', qv, bank, [bk], [qk])
        if not dry:
            PTs = [(PTx, [('PTx', 0), ('PTx', 1)]), (A[:, 16:18, :], [('A', 16), ('A', 17)])]

            def xa_a(hd):
                PTv, PTk = PTs[hd % 2]
                for mj in range(2):
                    bank, bk = pm_next()
                    for dc in range(2):
                        qv, qk = qT(hd * 2 + dc)
                        mm(bank, xaK[:, l, hd * 2 + dc, mj * 128:(mj + 1) * 128], qv, dc == 0, dc == 1, [('xaK', l), qk], [bk])
                    act(PTv[:, mj, :], bank, AF.Exp, [bk], [PTk[mj]], scale=1.0 / 16.0)

            def xa_b(hd):
                PTv, PTk = PTs[hd % 2]
                pa, pak = pb[6 + hd % 2], ('pb', 6 + hd % 2)
                for mj in range(2):
                    mm(pa, ones, PTv[:, mj, :], mj == 0, mj == 1, ['ones', PTk[mj]], [pak])
                f2, f2k = F(2 + hd % 2)
                P.emit('vector', lambda e, f2=f2, pa=pa: e.reciprocal(out=f2, in_=pa), [pak], [f2k])
                for dc in range(2):
                    bank, bk = pm_next()
                    for mj in range(2):
                        mm(bank, xaV[:, l, mj, hd * 256 + dc * 128: hd * 256 + (dc + 1) * 128], PTv[:, mj, :], mj == 0, mj == 1,
                           [('xaV', l), PTk[mj]], [bk])
                    ov, ok = oT(hd * 2 + dc)
                    tt('vector', ov, bank, f2, ALU.mult, [bk, f2k], [ok])

            xa_a(0); xa_a(1); xa_b(0); xa_a(2); xa_b(1); xa_a(3); xa_b(2); xa_b(3)
        resid_add(lambda nb, kg: f'wo{l}_{nb}', A[:, 8:16, :], [('A', 8 + c) for c in range(8)], [8])

    def ffn(l, gidx):
        dry = wstate['dry']
        if not dry:
            norm_T(xs, 4, gidx, hT, HTK, XK)
        for half, (p0, p1, groups) in enumerate([(0, 5, [8, 2]), (5, 11, [8, 4])]):
            for p_ in range(p0, p1):
                wv, wk = wnext(f'w13_{l}_{p_}')
                if dry:
                    continue
                for cc in range(2):
                    bg, bgk = proj_fm(wv, wk, cc * 128, None)
                    bu, buk = proj_fm(wv, wk, 256 + cc * 128, None)
                    f, fk = F(4 + (p_ * 2 + cc) % 4)
                    act(f, bg, AF.Silu, [bgk], [fk])
                    c = (p_ - p0) * 2 + cc
                    tt('vector', A[:, c, :], bu, f, ALU.mult, [buk, fk], [('A', c)])
            nch = sum(groups)
            resid_add(lambda nb, kg, half=half: f'w2_{l}_{half}_{nb}_{kg}', A[:, 0:nch, :], [('A', c) for c in range(nch)], groups)

    def mem_kv(l):
        ms = xs
        dma('sync', ms[:, 0:2, :], mem_d.rearrange("(j p) d -> p j d", p=128), (), [('x', 0), ('x', 1)])
        dma('sync', gall[:, 6, :], Wd['xa_mem_norm'][l].rearrange("(kc p) -> p kc", p=128), (), ['gall'], slow=True)
        norm_T(ms, 2, 6, hT, HTK, XK)
        for nb in range(4):
            wb, kcn, ncols = wblocks[f'wkv{l}_{nb}']
            dma('sync', WR[:, nb % 4, :, :], wb, [('wb', f'wkv{l}_{nb}')], [('W', nb % 4)])
        for oc in range(8):
            bank, bk = pm_next()
            for kc in range(8):
                mm(bank[:, 0:256], WR[:, oc // 4, kc, (oc % 4) * 128:(oc % 4 + 1) * 128], hT[:, kc, 0:256], kc == 0, kc == 7,
                   [('W', oc // 4)] + HTK, [bk])
            cp('scalar', xaK[:, l, oc, :], bank[:, 0:256], [bk], [('xaK', l)])
        for mj in range(2):
            for nb in range(2):
                bank, bk = pm_next()
                for kc in range(8):
                    mm(bank, hT[:, kc, mj * 128:(mj + 1) * 128], WR[:, 2 + nb, kc, :], kc == 0, kc == 7, [('W', 2 + nb)] + HTK, [bk])
                cp('scalar', xaV[:, l, mj, nb * 512:(nb + 1) * 512], bank, [bk], [('xaV', l)])

    def mark(label):
        if not wstate['dry']:
            MARKS.append((label, P.cnt['tensor'], P.cnt['vector'], P.cnt['scalar']))

    def tile_body(ti):
        dry = wstate['dry']
        mark('tile_start')
        mixf = lambda c: (A[:, c, :], ('A', c))
        if not dry:
            dma('sync', xs, x_d[ti * T:(ti + 1) * T, :].rearrange("(j p) d -> p j d", p=128), (), XK)
            norm_T(xs, 4, G_EMIX, hT, HTK, XK)
        mark('l0_norm_done')
        if STAGE >= 1:
            layer0_mixer_main()
        mark('l0_hgrn_done')
        if STAGE >= 2:
            conv_branch(mixf)
            resid_add(lambda nb, kg: f'eout{nb}', A[:, 0:8, :], [('A', c) for c in range(8)], [8])
        mark('l0_conv_eout_done')
        if STAGE >= 3:
            xattn(0, G_XA0)
        mark('l0_xa_done')
        if STAGE >= 4:
            ffn(0, G_FFN0)
        mark('l0_ffn_done')
        if nlayers > 1:
            layer1(ti)
            mark('l1_mixer_done')
            if os.environ.get('KSTOP', '0') == '1':
                return
            xattn(1, G_XA1)
            mark('l1_xa_done')
            ffn(1, G_FFN1)
            mark('l1_ffn_done')
        if not dry:
            for j in range(4):
                act(junk, xs[:, j, :], AF.Square, [('x', j)], JK + [('ss', j)], accum=ss[:, j:j + 1])
            act(rs, ss, AF.Sqrt, [('ss', j) for j in range(4)], ['rs'], scale=1.0 / D, bias=epsD)
            P.emit('vector', lambda e: e.reciprocal(out=rs, in_=rs), ['rs'], ['rs'])
            dma('sync', gfin, Wd['final_norm'].partition_broadcast(128), (), [('F', 0), ('F', 1)])
            for j in range(4):
                stt(xs[:, j, :], xs[:, j, :], rs[:, j:j + 1], gfin, ALU.mult, ALU.mult, [('x', j), 'rs', ('F', 0), ('F', 1)], [('x', j)])
            dma('sync', y_d[ti * T:(ti + 1) * T, :].rearrange("(j p) d -> p j d", p=128), xs, XK, [('y', ti)])

    def layer0_mixer_main():
        dry = wstate['dry']
        _l0()

    def _l0():
        dry = wstate['dry']
        qd = lambda h: (A[:, 0 + h, :], ('A', 0 + h))
        kd = lambda h: (A[:, 4 + h, :], ('A', 4 + h))
        kdl = lambda h: (A[:, 8 + h, :], ('A', 8 + h))
        og = lambda h: (A[:, 12 + h, :], ('A', 12 + h))
        mix = lambda c: (A[:, c, :], ('A', c))
        w_aq, k_aq = wnext('ein0')
        w_af, k_af = wnext('ein1', keep_prev=True)
        if not dry:
            for h in range(4):
                bank, bk = proj_fm(w_af, k_af, h * 128, None)
                f0, f0k = F(0); f1, f1k = F(1); f2, f2k = F(2); f3, f3k = F(3)
                f4, f4k = F(4); f5, f5k = F(5); f6, f6k = F(6)
                act(f0, bank, AF.Sigmoid, [bk], [f0k])
                ts('vector', f1, f0, oml[:, h:h + 1], lb[:, h:h + 1], ALU.mult, ALU.add, [f0k, 'oml', 'lb'], [f1k])
                act(f2, f1, AF.Ln, [f1k], [f2k])
                ts('vector', f3, f1, -1.0, 1.0, ALU.mult, ALU.add, [f1k], [f3k])
                P.emit('vector', lambda e, f4=f4, f2=f2: e.tensor_tensor_scan(out=f4, data0=rmask, data1=f2, initial=0.0,
                                                                             op0=ALU.mult, op1=ALU.add), [f2k, 'rmask'], [f4k])
                act(f5, f4, AF.Exp, [f4k], [f5k])
                act(f6, f4, AF.Exp, [f4k], [f6k], scale=-1.0)
                kdv, kdk = kd(h)
                tt('vector', kdv, f3, f6, ALU.mult, [f3k, f6k], [kdk])
                kdlv, kdlk = kdl(h)
                ebl_b = f5.rearrange("p (c s) -> p c s", s=64)[:, :, 63:64].to_broadcast([128, 8, 64])
                tt('vector', kdlv.rearrange("p (c s) -> p c s", s=64), kdv.rearrange("p (c s) -> p c s", s=64), ebl_b,
                   ALU.mult, [kdk, f5k], [kdlk])
                cp('gpsimd', EBL[:, h, :], f5.rearrange("p (c s) -> p c s", s=64)[:, :, 63], [f5k], [('EBL', h)])
                bank, bk = proj_fm(w_aq, k_aq, h * 128, None)
                act(f0, bank, AF.Silu, [bk], [f0k])
                qdv, qdk = qd(h)
                tt('vector', qdv, f0, f5, ALU.mult, [f0k, f5k], [qdk])
        w_ai, k_ai = wnext('ein2')
        if not dry:
            for j in range(4):
                bank, bk = pm_next()
                for kc in range(8):
                    mm(bank, hT[:, kc, j * 128:(j + 1) * 128], w_ai[:, kc, :], kc == 0, kc == 7, [k_ai] + HTK, [bk])
                cp('scalar', Vt[:, j, :], bank, [bk], [('Vt', j)])
        w_ag, k_ag = wnext('ein3')
        if not dry and SUB >= 2:
            for h in range(4):
                bank, bk = proj_fm(w_ag, k_ag, h * 128, None)
                ogv, ogk = og(h)
                act(ogv, bank, AF.Silu, [bk], [ogk])
            for j in range(4):
                p, pk = pt_next()
                for h in range(4):
                    kdlv, kdlk = kdl(h)
                    tr(p[:, h * 128:(h + 1) * 128], kdlv[:, j * 128:(j + 1) * 128], ident, [kdlk, 'ident'], [pk])
                cp('scalar', KTt[:, j, :], p[:, 0:512], [pk], [('KTt', j)])
            obanks = pm4()
            SK = [('S', h) for h in range(4)]
            SBK = [('Sbf', h) for h in range(4)]
            for j in range(4):
                sl = slice(j * 128, (j + 1) * 128)
                pa, pak = pb[6], ('pb', 6)
                ps7, ps7k = pb[7], ('pb', 7)
                for h in range(4):
                    mm(pa[:, h * 128:(h + 1) * 128], kd(h)[0][:, sl], qd(h)[0][:, sl], True, True, [kd(h)[1], qd(h)[1]], [pak])
                tt('vector', attn4, pa.rearrange("p (h t) -> p h t", t=128), mpair.unsqueeze(1).to_broadcast([128, 4, 128]), ALU.mult,
                   [pak, 'mpair'], ['attn4'])
                for h in range(4):
                    ob, obk = obanks[h]
                    vsl = slice(h * 128, (h + 1) * 128)
                    mm(ob[:, sl], Vt[:, j, vsl], attn4[:, h, :], True, False, [('Vt', j), 'attn4'], [obk])
                for cc in range(2):
                    ps = slice(cc * 64, (cc + 1) * 64)
                    tsl = slice(j * 128 + cc * 64, j * 128 + (cc + 1) * 64)
                    for h in range(4):
                        ob, obk = obanks[h]
                        mm(ob[:, tsl], Sbf[:, h, :], qd(h)[0][:, tsl], False, cc == 1, [('Sbf', h), qd(h)[1]], [obk])
                    for h in range(4):
                        vsl = slice(h * 128, (h + 1) * 128)
                        mm(ps7[:, vsl], KTt[ps, j, vsl], Vt[ps, j, vsl], True, True, [('KTt', j), ('Vt', j)], [ps7k])
                    for h in range(4):
                        vsl = slice(h * 128, (h + 1) * 128)
                        stt(Sst[:, h, :], Sst[:, h, :], EBL[:, h, j * 2 + cc:j * 2 + cc + 1], ps7[:, vsl],
                            ALU.mult, ALU.add, [('S', h), ('EBL', h), ps7k], [('S', h)])
                    cp('scalar', Sbf.rearrange("p h v -> p (h v)"), Sst.rearrange("p h v -> p (h v)"), SK, SBK)
            for h in range(4 if SUB >= 4 else 0):
                ob, obk = obanks[h]
                act(osq, ob, AF.Square, [obk], ['osq'])
                f1, f1k = F(1); f2, f2k = F(2)
                ogv, ogk = og(h)
                tt('vector', f1, ob, ogv, ALU.mult, [obk, ogk], [f1k])
                pa, pak = pb[6 + h % 2], ('pb', 6 + h % 2)
                mm(pa, ones, osq, True, True, ['ones', 'osq'], [pak])
                act(f2, pa, AF.Sqrt, [pak], [f2k], scale=1.0 / 128, bias=epsD)
                P.emit('vector', lambda e, f2=f2: e.reciprocal(out=f2, in_=f2), [f2k], [f2k])
                mv, mk = mix(h)
                stt(mv, f1, again[:, h:h + 1], f2, ALU.mult, ALU.mult, [f1k, 'again', f2k], [mk])


    if nlayers > 1:
        kidxT2 = sb("kidxT2", [128, S], BF16)
        ckvT = sb("ckvT", [128, S], BF16)
        ckvtok = sb("ckvtok", [128, S // 128, 128], BF16)
        score = sb("score", [128, S], BF16)
        mk = Fb.rearrange("p a t -> p (a t)").bitcast(BF16)
        FALL = [('F', i) for i in range(8)]
        wuq_sb = sb("wuq_sb", [128, 2, 512], BF16)
        iwq_sb = sb("iwq_sb", [128, 2, 512], BF16)
        wuk_sb = sb("wuk_sb", [128, 4, 128], BF16)
        wuv_sb = sb("wuv_sb", [128, 8, 64], BF16)
        widx_sb = sb("widx_sb", [128, 8, 8], BF16)
        wT_sb = sb("wT_sb", [128, 4, 128], BF16)
        wsl = sb("wsl", [128, 4, 128], BF16)
        bs_bc = sb("bs_bc", [128, 4, 128], BF16)
        vg_bc = sb("vg_bc", [128, 512], BF16)
        vb_bc = sb("vb_bc", [128, 512], BF16)
        cb = sb("cb", [128, 128], F32)
        cqg = sb("cqg", [128, 2], F32)
        ckvg = sb("ckvg", [128, 1], F32)
        kig = sb("kig", [128, 1], F32)
        kib = sb("kib", [128, 1], F32)
        eps5 = sb("eps5", [128, 1], F32)
        wraw = sb("wraw", [128, 4, 8], F32)
        wabs = sb("wabs", [128, 4, 8], F32)
        wsgn = sb("wsgn", [128, 4, 8], F32)
        sm = sb("sm", [128, 16], F32)
        lnst = sb("lnst", [128, 4, 4], F32)
        rden = sb("rden", [128, 8], F32)
        memset('vector', eps5, 1e-5, ['eps5'])
        dma('gpsimd', wuq_sb, Wd['odd_c_w_uq'][0].rearrange("(c p) n -> p c n", p=128), (), ['wuq'])
        dma('gpsimd', iwq_sb, Wd['odd_c_idx_wq'][0].rearrange("(c p) n -> p c n", p=128), (), ['iwq'])
        for hp in range(4):
            dma('gpsimd', wuk_sb[:, hp, :], Wd['odd_c_w_uk'][0][2 * hp:2 * hp + 2].rearrange("h d c -> (h d) c"), (), ['wuk'])
        dma('gpsimd', wuv_sb, Wd['odd_c_w_uv'][0].rearrange("h c d -> c h d"), (), ['wuv'])
        dma('gpsimd', widx_sb, Wd['odd_w_in'][0][:, 448:456].rearrange("(kc p) n -> p kc n", p=128), (), ['widx'], slow=True)
        dma('gpsimd', wsl, Wd['odd_d_w_s'][0].rearrange("g t s -> t g s"), (), ['wsl'])
        for g_ in range(4):
            P.emit('gpsimd', lambda e, g_=g_: e.affine_select(out=wsl[:, g_, :], in_=wsl[:, g_, :], pattern=[[-1, 128]], compare_op=ALU.is_ge,
                                                            fill=0.0, base=0, channel_multiplier=1), ['wsl'], ['wsl'])
        p_, pk_ = pt_next()
        for g_ in range(4):
            tr(p_[:, g_ * 128:(g_ + 1) * 128], wsl[:, g_, :], ident, ['wsl', 'ident'], [pk_])
        cp('vector', wT_sb, p_[:, 0:512].rearrange("p (g t) -> p g t", t=128), [pk_], ['wT'])
        dma('gpsimd', bs_bc.rearrange("p g t -> p (g t)"), Wd['odd_d_b_s'][0].rearrange("g t -> (g t)").partition_broadcast(128), (), ['bsbc'])
        dma('gpsimd', vg_bc, Wd['odd_d_v_g'][0].partition_broadcast(128), (), ['vgbc'])
        dma('gpsimd', vb_bc, Wd['odd_d_v_b'][0].partition_broadcast(128), (), ['vbbc'])
        memset('vector', cb, 0.0, ['cb'])
        P.emit('gpsimd', lambda e: e.affine_select(out=cb, in_=cb, pattern=[[-1, 128]], compare_op=ALU.is_ge,
                                                   fill=NEG, base=0, channel_multiplier=1), ['cb'], ['cb'])
        dma('sync', cqg, Wd['odd_c_q_norm'][0].rearrange("(c p) -> p c", p=128), (), ['cqg'], slow=True)
        dma('sync', ckvg, Wd['odd_c_kv_norm'][0].rearrange("(c p) -> p c", p=128), (), ['ckvg'], slow=True)
        for hh in range(2):
            dma('sync', kig[hh * 64:(hh + 1) * 64, :], Wd['odd_c_idx_k_g'][0].rearrange("(c p) -> p c", p=64), (), ['kig'], slow=True)
            dma('sync', kib[hh * 64:(hh + 1) * 64, :], Wd['odd_c_idx_k_b'][0].rearrange("(c p) -> p c", p=64), (), ['kib'], slow=True)

    NR = 16

    MBDT = mybir.dt.float8e4 if os.environ.get('KMB', 'fp8') == 'fp8' else BF16
    MBV = -224.0
    if nlayers > 1:
        if MBDT == BF16:
            assert S <= 4096
            mbv_ = Fb.rearrange("p a t -> p (a t)").bitcast(BF16)
            mbuf = [mbv_[:, 0:4096], mbv_[:, 4096:8192]]
        else:
            mbv_ = Fb.rearrange("p a t -> p (a t)").bitcast(MBDT)
            mbuf = [mbv_[:, 0:8192], mbv_[:, 8192:16384]]
        MBK = [[('F', i) for i in range(4)], [('F', 4 + i) for i in range(4)]]
        PTb = [hT[:, 0:2, :].rearrange("p a t -> p (a t)"), KTt[:, 0:2, :].rearrange("p a t -> p (a t)")]
        PTKb = [[('hT', 0), ('hT', 1)], [('KTt', 0), ('KTt', 1)]]
        acc_ = hT[:, 2:4, :].rearrange("p a t -> p (a t)").bitcast(F32)
        ACK = [('hT', 2), ('hT', 3)]
        Rh = [hT[:, 4, :], hT[:, 5, :]]
        RHK = [('hT', 4), ('hT', 5)]
        olat = hT[:, 6:8, :].rearrange("p a t -> p (a t)")
        OLK = [('hT', 6), ('hT', 7)]
        qlb = Vt[:, 0:2, :].rearrange("p a t -> p (a t)")
        QLK = [('Vt', 0), ('Vt', 1)]
        otok = Vt[:, 2, :]
        OTK = ('Vt', 2)
        ib_i = [0]
        qk_i = [0]

    def ib_next():
        i = ib_i[0] % 2
        ib_i[0] += 1
        return pb[i], ('pb', i)

    def qk_next():
        i = (2, 4, 5)[qk_i[0] % 3]
        qk_i[0] += 1
        return pb[i], ('pb', i)

    def dsa_indexer(ti, qbl):
        Q = ti * 4 + qbl
        L = (Q + 1) * 128
        qsl = slice(qbl * 128, (qbl + 1) * 128)
        nsb = (L + 511) // 512
        for sbi in range(nsb):
            n = min(512, L - sbi * 512)
            ksl = slice(sbi * 512, sbi * 512 + n)
            kk = [('kidx', sbi)]
            for h in range(8):
                bank, bk = ib_next()
                hp = slice((h % 2) * 64, (h % 2 + 1) * 64)
                mm(bank[:, 0:n], A[hp, 6 + h // 2, qsl], kidxT2[hp, ksl], True, True, [('A', 6 + h // 2)] + kk, [bk])
                r = Rh[h % 2]
                rk = RHK[h % 2]
                act(r[:, 0:n], bank[:, 0:n], AF.Relu, [bk, 'wabs'], [rk], scale=wabs[:, qbl, h:h + 1])
                if h == 0:
                    ts('vector', acc_[:, 0:n], r[:, 0:n], wsgn[:, qbl, 0:1], None, ALU.mult, None, [rk, 'wsgn'], ACK)
                elif h < 7:
                    stt(acc_[:, 0:n], r[:, 0:n], wsgn[:, qbl, h:h + 1], acc_[:, 0:n], ALU.mult, ALU.add, [rk, 'wsgn'] + ACK, ACK)
                else:
                    stt(score[:, ksl], r[:, 0:n], wsgn[:, qbl, h:h + 1], acc_[:, 0:n], ALU.mult, ALU.add, [rk, 'wsgn'] + ACK, ['score'])
        dsl = slice(Q * 128, (Q + 1) * 128)
        tt('vector', score[:, dsl], score[:, dsl], cb, ALU.add, ['score', 'cb'], ['score'])

    def dsa_threshold(ti, qbl):
        Q = ti * 4 + qbl
        L = (Q + 1) * 128
        mb = mbuf[Q % 2]
        mbk = MBK[Q % 2]
        lo = sm[:, 0:1]; hi = sm[:, 1:2]; w0_ = sm[:, 2:3]; mid = sm[:, 3:4]; cnt = sm[:, 4:5]; selw = sm[:, 5:6]
        if Q >= 2:
            P.emit('vector', lambda e: e.tensor_reduce(out=hi, in_=score[:, 0:L], axis=AX.X, op=ALU.max), ['score'], ['hi'])
            P.emit('vector', lambda e: e.tensor_reduce(out=lo, in_=score[:, 0:L - 128], axis=AX.X, op=ALU.min), ['score'], ['lo'])
            tt('vector', w0_, hi, lo, ALU.subtract, ['hi', 'lo'], ['w0'])
            for r_ in range(NR):
                sc_ = 2.0 ** -(r_ + 1)
                stt(mid, w0_, sc_, lo, ALU.mult, ALU.add, ['w0', 'lo'], ['mid'])
                ts('vector', mb[:, 0:L], score[:, 0:L], mid, 0.0, ALU.is_ge, ALU.add, ['score', 'mid'], mbk + ['cnt'], accum=cnt)
                ts('vector', selw, cnt, 256.0, sc_, ALU.is_ge, ALU.mult, ['cnt'], ['selw'])
                stt(lo, selw, w0_, lo, ALU.mult, ALU.add, ['selw', 'w0', 'lo'], ['lo'])
        else:
            memset('vector', lo, NEG * 0.5, ['lo'])
        ts('vector', mb[:, 0:L], score[:, 0:L], lo, MBV, ALU.is_lt, ALU.mult, ['score', 'lo'], mbk)

    def dsa_attention(ti, qbl):
        Q = ti * 4 + qbl
        qsl = slice(qbl * 128, (qbl + 1) * 128)
        mb = mbuf[Q % 2]
        mbk = MBK[Q % 2]
        qb0, qb0k = qk_next()
        qb1, qb1k = qk_next()
        for h in range(8):
            bank, bk = (qb0, qb0k) if h % 2 == 0 else (qb1, qb1k)
            hp = slice((h % 2) * 64, (h % 2 + 1) * 64)
            mm(bank[:, (h // 2) * 128:(h // 2 + 1) * 128], wuk_sb[hp, h // 2, :], A[hp, 2 + h // 2, qsl], True, True,
               ['wuk', ('A', 2 + h // 2)], [bk])
        qlv = qlb.rearrange("p (hp two t) -> p hp two t", two=2, t=128)
        act(qlv[:, :, 0, :], qb0.rearrange("p (h t) -> p h t", t=128), AF.Copy, [qb0k], QLK, scale=0.125)
        act(qlv[:, :, 1, :], qb1.rearrange("p (h t) -> p h t", t=128), AF.Copy, [qb1k], QLK, scale=0.125)
        accA = (pb[6], ('pb', 6)); accB = (pb[7], ('pb', 7)); accC = (pb[3], ('pb', 3))
        for st_ in range(Q + 1):
            ssl = slice(st_ * 128, (st_ + 1) * 128)
            ck = [('ckvT', st_ // 4)]
            PT = PTb[st_ % 2]
            PTK = PTKb[st_ % 2]
            lbs = [qk_next(), qk_next()]
            for half in range(2):
                lb_, lk_ = lbs[half]
                mm(lb_, ckvT[:, ssl], qlb[:, half * 512:(half + 1) * 512], True, False, ck + [QLK[half]], [lk_], sgc=True)
                for hh in range(4):
                    mm(lb_[:, hh * 128:(hh + 1) * 128], mb[:, ssl], ident, False, hh == 3, mbk + ['ident'], [lk_], sgc=True)
                act(PT[:, half * 512:(half + 1) * 512], lb_, AF.Exp, [lk_], [PTK[half]])
            ctk = [('ckvtok', st_ // 4)]
            first = (st_ == 0)
            last = (st_ == Q)
            mm(accA[0][:, 0:384], ckvtok[:, st_, :], PT[:, 0:384], first, last, ctk + PTK, [accA[1]])
            mm(accB[0][:, 0:384], ckvtok[:, st_, :], PT[:, 384:768], first, last, ctk + PTK, [accB[1]])
            mm(accC[0][:, 0:256], ckvtok[:, st_, :], PT[:, 768:1024], first, last, ctk + PTK, [accC[1]], sgc=True)
            for h in range(8):
                mm(accC[0][:, 256 + h:257 + h], PT[:, h * 128:(h + 1) * 128], ones[:, 0:1], False, last, PTK + ['ones'], [accC[1]], sgc=True)
        act(olat[:, 0:384], accA[0][:, 0:384], AF.Copy, [accA[1]], OLK)
        act(olat[:, 384:768], accB[0][:, 0:384], AF.Copy, [accB[1]], OLK)
        act(olat[:, 768:1024], accC[0][:, 0:256], AF.Copy, [accC[1]], OLK)
        P.emit('vector', lambda e, src_=accC[0][:, 256:264]: e.reciprocal(out=rden, in_=src_), [accC[1]], ['rden'])
        ob_, obk_ = qk_next()
        for h in range(8):
            mm(ob_[:, h * 64:(h + 1) * 64], olat[:, h * 128:(h + 1) * 128], wuv_sb[:, h, :], True, True, OLK + ['wuv'], [obk_])
        tt('vector', otok.rearrange("p (h d) -> p h d", d=64), ob_.rearrange("p (h d) -> p h d", d=64),
           rden.unsqueeze(2).to_broadcast([128, 8, 64]), ALU.mult, [obk_, 'rden'], [OTK])
        p, pk = qk_next()
        p = p.bitcast(BF16)
        for k_ in range(4):
            tr(p[:, k_ * 128:(k_ + 1) * 128], otok[:, k_ * 128:(k_ + 1) * 128], ident, [OTK, 'ident'], [pk])
        MK4 = [('A', 10 + c) for c in range(4)]
        cp('scalar', A[:, 10:14, qsl], p[:, 0:512].rearrange("p (k t) -> p k t", t=128), [pk], MK4)


    def layer1(ti):
        dry = wstate['dry']
        if not dry:
            norm_T(xs, 4, G_OMIX, hT, HTK, XK)
        w0, k0 = wnext('oin0')
        tsl_ = slice(ti * T, (ti + 1) * T)
        if not dry:
            sqb = [PTx[:, 0, :], PTx[:, 1, :], osq, A[:, 10, :]]
            sqk = [('PTx', 0), ('PTx', 1), 'osq', ('A', 10)]
            raw = [F(0), F(1), F(3), F(5)]
            cols = [0, 128, 256, 384]
            for i in range(4):
                bank, bk = proj_fm(w0, k0, cols[i], None)
                act(sqb[i], bank, AF.Square if i < 3 else AF.Copy, [bk], [sqk[i]])
                cp('vector', raw[i][0], bank, [bk], [raw[i][1]])
            pq, pqk = pb[6], ('pb', 6)
            pc, pck = pb[7], ('pb', 7)
            pi, pik = pb[3], ('pb', 3)
            for c in range(2):
                mm(pq, ones, sqb[c], c == 0, c == 1, ['ones', sqk[c]], [pqk])
            mm(pc, ones, sqb[2], True, True, ['ones', sqk[2]], [pck])
            mm(pi, ones, sqb[3], True, True, ['ones', sqk[3]], [pik])
            f2, f2k = F(2); f3, f3k = F(3); f4, f4k = F(4); f5, f5k = F(5); f6, f6k = F(6)
            act(f2, pq, AF.Sqrt, [pqk], [f2k], scale=1.0 / 256, bias=epsD)
            P.emit('vector', lambda e, f2=f2: e.reciprocal(out=f2, in_=f2), [f2k], [f2k])
            for c in range(2):
                f, fk = F(c)
                stt(A[:, c, :], f, cqg[:, c:c + 1], f2, ALU.mult, ALU.mult, [fk, 'cqg', f2k], [('A', c)])
            act(f4, pc, AF.Sqrt, [pck], [f4k], scale=1.0 / 128, bias=epsD)
            P.emit('vector', lambda e, f4=f4: e.reciprocal(out=f4, in_=f4), [f4k], [f4k])
            stt(ckvT[:, tsl_], f3, ckvg[:, 0:1], f4, ALU.mult, ALU.mult, [f3k, 'ckvg', f4k], [('ckvT', ti)])
            stt(f5, pi, -1.0 / 128, f5, ALU.mult, ALU.add, [pik, f5k], [f5k])
            act(A[:, 11, :], f5, AF.Square, [f5k], [('A', 11)])
            for j in range(4):
                bank, bk = pm_next()
                for kc in range(8):
                    mm(bank[:, 0:8], hT[:, kc, j * 128:(j + 1) * 128], widx_sb[:, kc, :], kc == 0, kc == 7, ['widx'] + HTK, [bk])
                cp('vector', wraw[:, j, :], bank[:, 0:8], [bk], [('wraw', j)])
            WRK = [('wraw', j) for j in range(4)]
            act(wabs, wraw, AF.Abs, WRK, ['wabs'], scale=(8.0 ** -0.5) * 0.125)
            act(wsgn, wraw, AF.Sign, WRK, ['wsgn'])
        w1, k1 = wnext('oin1')
        if not dry:
            for c in range(4):
                bank, bk = proj_fm(w1, k1, c * 128, None)
                act(A[:, 14 + c, :], bank, AF.Gelu, [bk], [('A', 14 + c)])
            mm(pi, ones, A[:, 11, :], True, True, ['ones', ('A', 11)], [pik])
            act(f6, pi, AF.Sqrt, [pik], [f6k], scale=1.0 / 128, bias=eps5)
            P.emit('vector', lambda e, f6=f6: e.reciprocal(out=f6, in_=f6), [f6k], [f6k])
            tt('vector', f5, f5, f6, ALU.mult, [f5k, f6k], [f5k])
            ts('vector', kidxT2[:, tsl_], f5, kig[:, 0:1], kib[:, 0:1], ALU.mult, ALU.add, [f5k, 'kig', 'kib'], [('kidx', ti)])
            p, pk = pt_next()
            for j in range(4):
                tr(p[:, j * 128:(j + 1) * 128], ckvT[:, ti * T + j * 128: ti * T + (j + 1) * 128], ident, [('ckvT', ti), 'ident'], [pk])
            cp('scalar', ckvtok[:, ti * 4:(ti + 1) * 4, :], p[:, 0:512].rearrange("p (j c) -> p j c", c=128), [pk], [('ckvtok', ti)])
            for oc in range(4):
                bank, bk = pm_next()
                for c in range(2):
                    mm(bank, wuq_sb[:, c, oc * 128:(oc + 1) * 128], A[:, c, :], c == 0, c == 1, ['wuq', ('A', c)], [bk])
                cp('scalar', A[:, 2 + oc, :], bank, [bk], [('A', 2 + oc)])
                bank, bk = pm_next()
                for c in range(2):
                    mm(bank, iwq_sb[:, c, oc * 128:(oc + 1) * 128], A[:, c, :], c == 0, c == 1, ['iwq', ('A', c)], [bk])
                cp('scalar', A[:, 6 + oc, :], bank, [bk], [('A', 6 + oc)])
        mark('l1_prep_done')
        w2_, k2 = wnext('oin2')
        if not dry and KL1 >= 2:
            for j in range(4):
                bank, bk = pm_next()
                for kc in range(8):
                    mm(bank, hT[:, kc, j * 128:(j + 1) * 128], w2_[:, kc, :], kc == 0, kc == 7, [k2] + HTK, [bk])
                f, fk = F(j % 2)
                g, gk = F(2 + j % 2)
                act(f, bank, AF.Gelu, [bk], [fk, ('lnst', j)], accum=lnst[:, j, 0:1])
                ts('vector', lnst[:, j, 1:2], lnst[:, j, 0:1], -1.0 / 512, None, ALU.mult, None, [('lnst', j)], [('lnst', j)])
                ts('vector', f, f, lnst[:, j, 1:2], None, ALU.add, None, [fk, ('lnst', j)], [fk])
                act(g, f, AF.Square, [fk], [gk, ('lnst', j)], accum=lnst[:, j, 2:3])
                act(lnst[:, j, 3:4], lnst[:, j, 2:3], AF.Sqrt, [('lnst', j)], [('lnst', j)], scale=1.0 / 512, bias=eps5)
                P.emit('vector', lambda e, j=j: e.reciprocal(out=lnst[:, j, 3:4], in_=lnst[:, j, 3:4]), [('lnst', j)], [('lnst', j)])
                stt(g, f, lnst[:, j, 3:4], vg_bc, ALU.mult, ALU.mult, [fk, ('lnst', j), 'vgbc'], [gk])
                tt('vector', KTt[:, j, :], g, vb_bc, ALU.add, [gk, 'vbbc'], [('KTt', j)])
                bank, bk = pm_next()
                for g_ in range(4):
                    mm(bank[:, g_ * 128:(g_ + 1) * 128], KTt[:, j, g_ * 128:(g_ + 1) * 128], wT_sb[:, g_, :], True, True, [('KTt', j), 'wT'], [bk])
                tt('vector', f, bank, bs_bc.rearrange("p g t -> p (g t)"), ALU.add, [bk, 'bsbc'], [fk])
                av = A[:, 14:18, j * 128:(j + 1) * 128]
                AK = [('A', 14 + c) for c in range(4)]
                tt('vector', av, f.rearrange("p (g t) -> p g t", t=128), av, ALU.mult, [fk] + AK, AK)
            mark('l1_gmlp_done')
            order = ['i0', 't0', 'i1', 't1', 'a0', 'i2', 't2', 'a1', 'i3', 't3', 'a2', 'a3']
            if os.environ.get('KSEQ', '0') == '1':
                order = ['i0', 't0', 'a0', 'i1', 't1', 'a1', 'i2', 't2', 'a2', 'i3', 't3', 'a3']
            for step in order:
                qbl = int(step[1])
                if KL1 < 3 or (KL1 == 3 and step[0] != 'i') or (KL1 == 4 and step[0] == 'a'):
                    continue
                if step[0] == 'i':
                    dsa_indexer(ti, qbl)
                elif step[0] == 't':
                    dsa_threshold(ti, qbl)
                else:
                    dsa_attention(ti, qbl)
        mark('l1_dsa_done')
        if os.environ.get('KSTOP', '0') == '1':
            return
        resid_add(lambda nb, kg: f'oout{nb}', A[:, 10:18, :], [('A', 10 + c) for c in range(8)], [8])

    gfin = Fb[:, 0:2, :].rearrange("p a t -> p (a t)")

    wstate['dry'] = True
    wstate['pos'] = 0
    for ti in range(NT):
        tile_body(ti)
    wstate['dry'] = False
    wstate['pos'] = 0
    dmy = sb("dmy", [128, 4], F32)
    P.barrier({
        'vector': (lambda e: e.memset(dmy[:, 0:1], 0.0), ['dmy0']),
        'gpsimd': (lambda e: e.memset(dmy[:, 1:2], 0.0), ['dmy1']),
        'scalar': (lambda e: e.activation(out=dmy[:, 2:3], in_=epsD, func=AF.Copy), ['dmy2']),
        'tensor': (lambda e: e.matmul(pb[0][0:1, 0:1], lhsT=ones[:, 0:1], rhs=ones[:, 0:1], start=True, stop=True), [('pb', 0)]),
    })
    emit_casts([f'wkv{l}_{nb}' for l in range(nlayers) for nb in range(4)])
    emit_casts(list(wseq))
    pending.clear()
    for l in range(nlayers):
        mem_kv(l)
    for ti in range(NT):
        tile_body(ti)
    P.finalize()
    return nc


_CACHE = {}
MARKS = []


def kernel(**inputs):
    x = np.ascontiguousarray(inputs['x'], dtype=np.float32)
    mem = np.ascontiguousarray(inputs['mem'], dtype=np.float32)
    B, S, _ = x.shape
    if S not in _CACHE:
        _CACHE[S] = build(S)
    nc = _CACHE[S]
    in_maps = []
    for b in range(B):
        m = {"x": x[b], "mem": mem[b]}
        for name, shp in WEIGHT_SPECS:
            m[name] = np.ascontiguousarray(inputs[name], dtype=np.float32)
        in_maps.append(m)
    res = run_bass_kernel_spmd(nc, in_maps, core_ids=list(range(B)))
    return np.stack([r["y"] for r in res.results], axis=0)
```
